# Optimizing a Trainium2 kernel written in Bass

```python
import math
import jax, jax.numpy as jnp
from jax import lax
import numpy as np

D_MODEL = 1024
BATCH = 1
SEQ = 16384
DEPTH = 1

PLE_DIM = 256
D_MIX = D_MODEL
DA_HEADS = 4
DA_HEAD_DIM = 64
DA_WIDTH = DA_HEADS * 2 * DA_HEAD_DIM
Q_BLOCK = 128
ML_HEADS = 4
ML_WIDTH = D_MIX - DA_WIDTH
ML_HEAD_DIM = ML_WIDTH // ML_HEADS
ML_CHUNK = 64
CONV_WIDTH = 4
IN_WIDTHS = [DA_WIDTH, DA_WIDTH, DA_WIDTH,
             ML_WIDTH, ML_WIDTH, ML_WIDTH, ML_WIDTH,
             ML_HEADS, ML_HEADS]
N_IN = sum(IN_WIDTHS)
IN_SPLITS = [int(s) for s in np.cumsum(IN_WIDTHS)[:-1]]
N_KEYS = 128
N_EXPERTS = N_KEYS * N_KEYS
PEER_HEADS = 8
PEER_TOPK = 16
PEER_QUERY_DIM = 256
PEER_HALF_DIM = PEER_QUERY_DIM // 2
PEER_BLOCK = 128
NORM_EPS = 1e-6

kernel_name = "hymba_diffattn_mlstm_peer_block"


def rms_norm(x, g):
    xf = x.astype(jnp.float32)
    y = xf * lax.rsqrt(jnp.mean(xf * xf, axis=-1, keepdims=True) + NORM_EPS)
    return (y * g.astype(jnp.float32)).astype(x.dtype)


def alibi_slopes(n):
    def pow2_slopes(m):
        start = 2.0 ** (-8.0 / m)
        return [start ** (i + 1) for i in range(m)]
    if math.log2(n).is_integer():
        s = pow2_slopes(n)
    else:
        c = 2 ** math.floor(math.log2(n))
        s = pow2_slopes(c) + pow2_slopes(2 * c)[0::2][: n - c]
    return jnp.asarray(np.array(s, dtype=np.float32))


def causal_dwconv(x, w, b):
    c = x.shape[-1]
    y = lax.conv_general_dilated(x, w[:, None, :].astype(x.dtype), window_strides=(1,),
                                 padding=[(CONV_WIDTH - 1, 0)],
                                 dimension_numbers=('NWC', 'WIO', 'NWC'),
                                 feature_group_count=c)
    return y + b.astype(x.dtype)


def diff_attention(q, k, v, lam, subln_g, lambda_init):
    B, S = q.shape[0], q.shape[1]
    nb = S // Q_BLOCK
    scale = DA_HEAD_DIM ** -0.5
    slopes = alibi_slopes(DA_HEADS)
    kpos = jnp.arange(S)
    qb = q.reshape(B, nb, Q_BLOCK, DA_HEADS, 2, DA_HEAD_DIM).transpose(1, 0, 2, 3, 4, 5)

    def block(args):
        qblk, bi = args
        qpos = bi * Q_BLOCK + jnp.arange(Q_BLOCK)
        s = jnp.einsum('bqhcd,bkhcd->bhcqk', qblk, k,
                       preferred_element_type=jnp.float32) * scale
        dist = (qpos[:, None] - kpos[None, :]).astype(jnp.float32)
        bias = -slopes[:, None, None, None] * dist
        s = jnp.where(dist >= 0, s + bias, -jnp.inf)
        pr = jax.nn.softmax(s, axis=-1)
        w = pr[:, :, 0] - lam * pr[:, :, 1]
        return jnp.einsum('bhqk,bkhd->bqhd', w.astype(v.dtype), v)

    o = lax.map(block, (qb, jnp.arange(nb)))
    o = o.transpose(1, 0, 2, 3, 4).reshape(B, S, DA_HEADS, 2 * DA_HEAD_DIM)
    o = rms_norm(o, subln_g) * (1.0 - lambda_init)
    return o.reshape(B, S, DA_WIDTH)


def mlstm_chunkwise(q, k, v, i_pre, f_pre):
    B, S, H, d = q.shape
    L = ML_CHUNK
    nc = S // L
    f32 = jnp.float32

    def chunks(a):
        return a.astype(f32).reshape(B, nc, L, H, d).transpose(1, 0, 3, 2, 4)

    def gchunks(a):
        return a.astype(f32).reshape(B, nc, L, H).transpose(1, 0, 3, 2)

    qc_all = chunks(q) * (d ** -0.5)
    kc_all, vc_all = chunks(k), chunks(v)
    logf_all = gchunks(jax.nn.log_sigmoid(f_pre.astype(f32)))
    ig_all = gchunks(i_pre)
    causal = jnp.tril(jnp.ones((L, L), dtype=bool))

    def step(carry, xs):
        C, n, m = carry
        qc, kc, vc, lf, ig = xs
        b = jnp.cumsum(lf, axis=-1)
        D = jnp.where(causal, b[..., :, None] - b[..., None, :] + ig[..., None, :], -jnp.inf)
        inter = b + m[..., None]
        m_t = jnp.maximum(inter, jnp.max(D, axis=-1))
        Dw = jnp.exp(D - m_t[..., None])
        a_inter = jnp.exp(inter - m_t)
        sqk = jnp.einsum('bhld,bhsd->bhls', qc, kc) * Dw
        num = a_inter[..., None] * jnp.einsum('bhld,bhde->bhle', qc, C) + \
            jnp.einsum('bhls,bhse->bhle', sqk, vc)
        den = a_inter * jnp.einsum('bhld,bhd->bhl', qc, n) + jnp.sum(sqk, axis=-1)
        h = num / jnp.maximum(jnp.abs(den), jnp.exp(-m_t))[..., None]
        bL = b[..., -1]
        g = bL[..., None] - b + ig
        m_new = jnp.maximum(bL + m, jnp.max(g, axis=-1))
        decay = jnp.exp(bL + m - m_new)
        w = jnp.exp(g - m_new[..., None])
        C_new = decay[..., None, None] * C + jnp.einsum('bhsd,bhse->bhde', kc * w[..., None], vc)
        n_new = decay[..., None] * n + jnp.einsum('bhs,bhsd->bhd', w, kc)
        return (C_new, n_new, m_new), h

    init = (jnp.zeros((B, H, d, d), f32), jnp.zeros((B, H, d), f32), jnp.zeros((B, H), f32))
    _, hs = lax.scan(step, init, (qc_all, kc_all, vc_all, logf_all, ig_all))
    return hs.transpose(1, 0, 3, 2, 4).reshape(B, S, H, d)


def peer_ffn(x, w_query, sub_keys, expert_u, expert_v):
    B, S, D = x.shape
    T = B * S
    nb = T // PEER_BLOCK
    xt = x.reshape(nb, PEER_BLOCK, D)

    def block(xb):
        q = (xb @ w_query).reshape(PEER_BLOCK, PEER_HEADS, 2, PEER_HALF_DIM)
        s = jnp.einsum('thcd,hcnd->thcn', q, sub_keys,
                       preferred_element_type=jnp.float32)
        sv, si = lax.top_k(s, PEER_TOPK)
        cand = (sv[:, :, 0, :, None] + sv[:, :, 1, None, :]).reshape(PEER_BLOCK, PEER_HEADS, -1)
        cidx = (si[:, :, 0, :, None] * N_KEYS + si[:, :, 1, None, :]).reshape(PEER_BLOCK, PEER_HEADS, -1)
        top_s, pos = lax.top_k(cand, PEER_TOPK)
        eidx = jnp.take_along_axis(cidx, pos, axis=-1)
        g = jax.nn.softmax(top_s, axis=-1)
        u = expert_u[eidx]
        a = jax.nn.gelu(jnp.einsum('td,thkd->thk', xb, u), approximate=False)
        vv = expert_v[eidx]
        return jnp.einsum('thk,thkd->td', (g * a).astype(x.dtype), vv)

    return lax.map(block, xt).reshape(B, S, D)


def setup_inputs(seed: int = 0) -> dict:
    key = jax.random.key(seed)
    ks = jax.random.split(key, 32)
    f32 = jnp.float32

    def nrm(k, shape, scale):
        return jax.random.normal(k, shape, f32) * scale

    return {
        'x': nrm(ks[0], (BATCH, SEQ, D_MODEL), 1.0),
        'p': nrm(ks[1], (DEPTH, BATCH, SEQ, PLE_DIM), 1.0),
        'norm_mix_g': 1.0 + nrm(ks[2], (DEPTH, D_MODEL), 0.02),
        'w_in': nrm(ks[3], (DEPTH, D_MODEL, N_IN), D_MODEL ** -0.5),
        'conv_w': nrm(ks[4], (DEPTH, CONV_WIDTH, 2 * ML_WIDTH), CONV_WIDTH ** -0.5),
        'conv_b': nrm(ks[5], (DEPTH, 2 * ML_WIDTH), 0.02),
        'b_igate': nrm(ks[6], (DEPTH, ML_HEADS), 0.1),
        'b_fgate': jnp.linspace(3.0, 6.0, ML_HEADS, dtype=f32)[None, :] + nrm(ks[7], (DEPTH, ML_HEADS), 0.1),
        'lambda_q1': nrm(ks[8], (DEPTH, DA_HEAD_DIM), 0.1),
        'lambda_k1': nrm(ks[9], (DEPTH, DA_HEAD_DIM), 0.1),
        'lambda_q2': nrm(ks[10], (DEPTH, DA_HEAD_DIM), 0.1),
        'lambda_k2': nrm(ks[11], (DEPTH, DA_HEAD_DIM), 0.1),
        'da_subln_g': 1.0 + nrm(ks[12], (DEPTH, 2 * DA_HEAD_DIM), 0.02),
        'ml_norm_g': 1.0 + nrm(ks[13], (DEPTH, ML_HEADS, ML_HEAD_DIM), 0.02),
        'w_out': nrm(ks[14], (DEPTH, D_MIX, D_MODEL), D_MIX ** -0.5),
        'norm_ffn_g': 1.0 + nrm(ks[15], (DEPTH, D_MODEL), 0.02),
        'w_query': nrm(ks[16], (DEPTH, D_MODEL, PEER_HEADS * PEER_QUERY_DIM), D_MODEL ** -0.5),
        'sub_keys': nrm(ks[17], (DEPTH, PEER_HEADS, 2, N_KEYS, PEER_HALF_DIM), PEER_HALF_DIM ** -0.5),
        'expert_u': nrm(ks[18], (DEPTH, N_EXPERTS, D_MODEL), D_MODEL ** -0.5),
        'expert_v': nrm(ks[19], (DEPTH, N_EXPERTS, D_MODEL), PEER_HEADS ** -0.5),
        'norm_ple_g': 1.0 + nrm(ks[20], (DEPTH, D_MODEL), 0.02),
        'w_ple_gate': nrm(ks[21], (DEPTH, D_MODEL, D_MODEL), D_MODEL ** -0.5),
        'w_ple_proj': nrm(ks[22], (DEPTH, PLE_DIM, D_MODEL), PLE_DIM ** -0.5),
        'final_norm_g': 1.0 + nrm(ks[23], (D_MODEL,), 0.02),
    }


def reference(x, p, norm_mix_g, w_in, conv_w, conv_b, b_igate, b_fgate,
              lambda_q1, lambda_k1, lambda_q2, lambda_k2, da_subln_g, ml_norm_g, w_out,
              norm_ffn_g, w_query, sub_keys, expert_u, expert_v,
              norm_ple_g, w_ple_gate, w_ple_proj, final_norm_g):
    B, S, _ = x.shape
    for i in range(DEPTH):
        lambda_init = 0.8 - 0.6 * math.exp(-0.3 * i)
        h = rms_norm(x, norm_mix_g[i])
        proj = h @ w_in[i]
        da_q, da_k, da_v, ml_q, ml_k, ml_v, ml_o, ml_i, ml_f = jnp.split(proj, IN_SPLITS, axis=-1)
        lam = (jnp.exp(jnp.sum(lambda_q1[i] * lambda_k1[i]).astype(jnp.float32))
               - jnp.exp(jnp.sum(lambda_q2[i] * lambda_k2[i]).astype(jnp.float32)) + lambda_init)
        da_out = diff_attention(
            da_q.reshape(B, S, DA_HEADS, 2, DA_HEAD_DIM),
            da_k.reshape(B, S, DA_HEADS, 2, DA_HEAD_DIM),
            da_v.reshape(B, S, DA_HEADS, 2 * DA_HEAD_DIM),
            lam, da_subln_g[i], lambda_init)
        qk = jax.nn.silu(causal_dwconv(jnp.concatenate([ml_q, ml_k], axis=-1), conv_w[i], conv_b[i]))
        mq, mk = jnp.split(qk, 2, axis=-1)
        hs = mlstm_chunkwise(
            mq.reshape(B, S, ML_HEADS, ML_HEAD_DIM),
            mk.reshape(B, S, ML_HEADS, ML_HEAD_DIM),
            ml_v.reshape(B, S, ML_HEADS, ML_HEAD_DIM),
            ml_i + b_igate[i], ml_f + b_fgate[i]).astype(x.dtype)
        o_gate = jax.nn.sigmoid(ml_o).reshape(B, S, ML_HEADS, ML_HEAD_DIM)
        ml_out = rms_norm(o_gate * hs, ml_norm_g[i]).reshape(B, S, ML_WIDTH)
        x = x + jnp.concatenate([da_out, ml_out], axis=-1) @ w_out[i]
        x = x + peer_ffn(rms_norm(x, norm_ffn_g[i]), w_query[i], sub_keys[i], expert_u[i], expert_v[i])
        gate = jax.nn.sigmoid(rms_norm(x, norm_ple_g[i]) @ w_ple_gate[i])
        x = x + gate * (p[i] @ w_ple_proj[i])
    return rms_norm(x, final_norm_g)
```

```python
import math
from contextlib import ExitStack
import numpy as np
import ml_dtypes
import concourse.bass as bass
import concourse.mybir as mybir
from concourse.bass_utils import run_bass_kernel_spmd

F32 = mybir.dt.float32
BF16 = mybir.dt.bfloat16
AF = mybir.ActivationFunctionType
ALU = mybir.AluOpType

ENGS = ["tensor", "vector", "scalar", "gpsimd", "sync"]
SAME_ENGINE_SYNC = {"vector": True, "scalar": True, "gpsimd": True, "tensor": False, "sync": False}

D = 1024
T_OWN = 2048
NQB = 16
NEG = -30000.0


class Sched:
    def __init__(self, nc):
        self.nc = nc
        self.lists = {e: [] for e in ENGS}
        self.esem = {e: nc.alloc_semaphore("es_" + e) for e in ENGS if e != "sync"}
        self.cnt = {e: 0 for e in ENGS}
        self.seen = {e: {} for e in ENGS}
        self.writer = {}
        self.readers = {}
        self.dsem = {}
        self.n_dma_sems = 0
        self.nops = 0

    def _wait(self, eng, dep):
        sem, val, src = dep
        if src == eng and not SAME_ENGINE_SYNC.get(eng, False):
            return
        k = id(sem)
        if self.seen[eng].get(k, 0) >= val:
            return
        self.seen[eng][k] = val
        self.lists[eng].append(("wait", sem, val))

    def _deps(self, eng, reads, writes):
        for r in reads:
            for d in self.writer.get(r, {}).values():
                self._wait(eng, d)
            if len(r) > 1 and r[0] == "p" and r[1].isupper():
                for d in self.readers.get(r, ()):
                    if d[2] != eng:
                        self._wait(eng, d)
        for w in writes:
            for d in self.writer.get(w, {}).values():
                self._wait(eng, d)
            for d in self.readers.get(w, ()):
                self._wait(eng, d)

    def _commit(self, dep, reads, writes, merge=False):
        for w in writes:
            if merge:
                self.writer.setdefault(w, {})[id(dep[0])] = dep
            else:
                self.writer[w] = {id(dep[0]): dep}
            self.readers[w] = []
        for r in reads:
            lst = self.readers.setdefault(r, [])
            lst[:] = [d for d in lst if d[0] is not dep[0]]
            lst.append(dep)

    def op(self, eng, fn, reads=(), writes=()):
        self._deps(eng, reads, writes)
        self.cnt[eng] += 1
        self.nops += 1
        dep = (self.esem[eng], self.cnt[eng], eng)
        self.lists[eng].append(("op", fn, self.esem[eng], 1))
        self._commit(dep, reads, writes)

    DRAM_KEYS = ("KTscr", "Vscr", "Yscr", "X1scr", "Wscr", "out")

    def dma(self, fn, reads=(), writes=(), key=None, eng="sync"):
        self._deps(eng, reads, writes)
        if key is None:
            key = reads[0] if (writes and writes[0] in self.DRAM_KEYS and reads) else (writes[0] if writes else reads[0])
        if key not in self.dsem:
            self.dsem[key] = [self.nc.alloc_semaphore("ds%d" % self.n_dma_sems), 0]
            self.n_dma_sems += 1
        ds = self.dsem[key]
        ds[1] += 16
        self.nops += 1
        dep = (ds[0], ds[1], None)
        self.lists[eng].append(("op", fn, ds[0], 16))
        self._commit(dep, reads, writes, merge=True)

    def drain(self):
        for e in ENGS:
            for e2 in ENGS:
                if e2 != "sync" and self.cnt[e2] > 0 and (e2 != e):
                    self._wait(e, (self.esem[e2], self.cnt[e2], e2))
            for ds in self.dsem.values():
                self._wait(e, (ds[0], ds[1], None))

    def final_wait(self, eng, keys):
        for k in keys:
            for d in self.writer.get(k, {}).values():
                self._wait(eng, d)

    def emit(self):
        with self.nc.Block() as block:
            for e in ENGS:
                lst = self.lists[e]
                if not lst:
                    continue

                def body(engh, lst=lst):
                    for it in lst:
                        if it[0] == "wait":
                            engh.wait_ge(it[1], it[2])
                        else:
                            it[1](engh).then_inc(it[2], it[3])

                getattr(block, e)(body)
        self.lists = {e: [] for e in ENGS}


class K:
    def __init__(self, nc, S):
        self.nc, self.S = nc, S

    def mm(self, out, lhsT, rhs, r, w, start=True, stop=True):
        self.S.op("tensor", lambda e: e.matmul(out, lhsT=lhsT, rhs=rhs, start=start, stop=stop,
                                               skip_group_check=True), r, w)

    def tr(self, out, in_, ident, r, w):
        self.S.op("tensor", lambda e: e.transpose(out=out, in_=in_, identity=ident), r, w)

    def act(self, out, in_, func, r, w, **kw):
        self.S.op("scalar", lambda e: e.activation(out=out, in_=in_, func=func, **kw), r, w)

    def ts(self, eng, out, in0, s1, s2, op0, op1, r, w):
        if op1 is None:
            self.S.op(eng, lambda e: e.tensor_scalar(out=out, in0=in0, scalar1=s1, scalar2=None, op0=op0), r, w)
        else:
            self.S.op(eng, lambda e: e.tensor_scalar(out=out, in0=in0, scalar1=s1, scalar2=s2, op0=op0, op1=op1), r, w)

    def tt(self, eng, out, in0, in1, op, r, w):
        self.S.op(eng, lambda e: e.tensor_tensor(out=out, in0=in0, in1=in1, op=op), r, w)

    def stt(self, out, in0, scalar, in1, op0, op1, r, w):
        self.S.op("vector", lambda e: e.scalar_tensor_tensor(out=out, in0=in0, scalar=scalar, in1=in1,
                                                             op0=op0, op1=op1), r, w)

    def cp(self, eng, out, in_, r, w):
        if eng == "scalar":
            self.S.op(eng, lambda e: e.copy(out=out, in_=in_), r, w)
        else:
            self.S.op(eng, lambda e: e.tensor_copy(out=out, in_=in_), r, w)

    def dma(self, out, in_, r, w, key=None, eng="sync"):
        self.S.dma(lambda e: e.dma_start(out=out, in_=in_), r, w, key=key, eng=eng)

    def memset(self, eng, out, val, w):
        self.S.op(eng, lambda e: e.memset(out, val), (), w)


C_QDA, C_KDA, C_VDA, C_MLQ, C_MLK, C_MLV, C_MLO, C_G = 0, 512, 1024, 1536, 2048, 2560, 3072, 3584
SLOPES = [2.0 ** (-2.0 * (i + 1)) for i in range(4)]
LAMBDA_INIT = 0.8 - 0.6 * math.exp(0.0)


def build(T_ALL, stage=99):
    NBLK = T_ALL // 128
    NT = T_ALL // 512
    OWN_BLK0 = NBLK - NQB
    OWN_T0 = NT - 4
    nc = bass.Bass("TRN2", target_bir_lowering=False)

    def din(name, shape, dt=F32):
        return nc.dram_tensor(name, list(shape), dt, kind="ExternalInput").ap()

    xseq = din("xseq", [T_ALL, D])
    p_own = din("p_own", [T_OWN, 256])
    padrow = din("padrow", [1, T_ALL], BF16)
    valid = din("valid", [128, NBLK])
    w_in = din("w_in", [D, 3592])
    norm_mix_g = din("norm_mix_g", [D])
    conv_w = din("conv_w", [128, 8, 4])
    conv_b = din("conv_b", [128, 8])
    b_gates = din("b_gates", [8])
    lam4 = din("lam4", [4, 64])
    da_subln_g = din("da_subln_g", [128])
    ml_norm_g = din("ml_norm_g", [512])
    w_out = din("w_out", [D, D])
    norm_ffn_g = din("norm_ffn_g", [D])
    w_query = din("w_query", [D, 2048])
    sub_keys = din("sub_keys", [16, 128, 128])
    expert_u = din("expert_u", [16384, D])
    expert_v = din("expert_v", [16384, D])
    norm_ple_g = din("norm_ple_g", [D])
    w_ple_gate = din("w_ple_gate", [D, D])
    w_ple_proj = din("w_ple_proj", [256, D])
    final_norm_g = din("final_norm_g", [D])
    c_identb = din("c_identb", [128, 128], BF16)
    c_identf = din("c_identf", [128, 128])
    c_tri = din("c_tri", [128, 128])
    c_qaug = din("c_qaug", [3, 4, T_OWN], BF16)
    c_bias = din("c_bias", [128, 4, 4, NBLK])
    c_dmask = din("c_dmask", [128, 4, 512])
    out = nc.dram_tensor("out", [T_OWN, D], F32, kind="ExternalOutput").ap()

    KTscr = nc.dram_tensor("KTscr", [4, 128, T_ALL], BF16).ap()
    Vscr = nc.dram_tensor("Vscr", [4, 128, NBLK, 128], BF16).ap()
    Yscr = nc.dram_tensor("Yscr", [T_OWN, D], BF16).ap()
    X1scr = nc.dram_tensor("X1scr", [T_OWN, D], F32).ap()

    S = Sched(nc)
    k = K(nc, S)

    with ExitStack() as G:
        def SB(ctx, name, shape, dt=F32):
            return ctx.enter_context(nc.sbuf_tensor(name, list(shape), dt))

        def PS(ctx, name, shape, dt=F32):
            return ctx.enter_context(nc.psum_tensor(name, list(shape), dt))

        identb = SB(G, "identb", [128, 128], BF16)
        identf = SB(G, "identf", [128, 128])
        tri = SB(G, "tri", [128, 128])
        ntri = SB(G, "ntri", [128, 128])
        ones = SB(G, "ones", [128, 128])
        P12 = ExitStack()
        QT0 = SB(P12, "QT0", [67, 4, T_OWN], BF16)
        QT1 = SB(P12, "QT1", [67, 4, T_OWN], BF16)
        lamc = SB(P12, "lamc", [128, 1])
        k.dma(identb[:], c_identb, [], ["identb"])
        k.dma(identf[:], c_identf, [], ["identf"])
        k.dma(tri[:], c_tri, [], ["tri"])
        k.memset("vector", ones[:], 1.0, ["ones"])
        k.ts("vector", ntri[:], tri[:], -1.0, None, ALU.add, None, ["tri"], ["ntri"])
        k.dma(QT0[64:67, :, :], c_qaug, [], ["QT0"])
        k.dma(QT1[64:67, :, :], c_qaug, [], ["QT1"])

        with ExitStack() as P1:
            Wb = SB(P1, "Wb", [128, 8, 3592], BF16)
            gmix = SB(P1, "gmix", [128, D])
            cw = SB(P1, "cw", [128, 8, 4])
            cb = SB(P1, "cb", [128, 8])
            bg = SB(P1, "bg", [128, 8])
            vld = SB(P1, "vld", [128, NBLK])
            gml = SB(P1, "gml", [128, 512])
            gsub = SB(P1, "gsub", [128, 128])
            lamt = SB(P1, "lamt", [128, 4, 64])
            lamp = SB(P1, "lamp", [128, 2, 64])
            lams = SB(P1, "lams", [128, 2])
            State = SB(P1, "State", [128, 4, 129])
            k.dma(gmix[:], norm_mix_g.partition_broadcast(128), [], ["gmix"])
            k.dma(cw[:], conv_w, [], ["cw"])
            k.dma(cb[:], conv_b, [], ["cb"])
            k.dma(bg[:], b_gates.partition_broadcast(128), [], ["bg"])
            k.dma(vld[:], valid, [], ["vld"])
            k.dma(gml[:], ml_norm_g.partition_broadcast(128), [], ["gml"])
            k.dma(gsub[:], da_subln_g.partition_broadcast(128), [], ["gsub"])
            k.dma(lamt[:], lam4.partition_broadcast(128), [], ["lamt"])
            k.memset("vector", State[:], 0.0, ["State"])
            k.tt("vector", lamp[:, 0, :], lamt[:, 0, :], lamt[:, 1, :], ALU.mult, ["lamt"], ["lamp"])
            k.tt("vector", lamp[:, 1, :], lamt[:, 2, :], lamt[:, 3, :], ALU.mult, ["lamt"], ["lamp"])
            S.op("vector", lambda e: e.reduce_sum(out=lams[:], in_=lamp[:], axis=mybir.AxisListType.X), ["lamp"], ["lams"])
            k.act(lams[:], lams[:], AF.Exp, ["lams"], ["lams"])
            k.tt("vector", lamc[:], lams[:, 1:2], lams[:, 0:1], ALU.subtract, ["lams"], ["lamc"])
            k.ts("vector", lamc[:], lamc[:], -LAMBDA_INIT, None, ALU.add, None, ["lamc"], ["lamc"])

            with ExitStack() as W0:
                wst = [SB(W0, "wst%d" % i, [128, 8, 512]) for i in range(2)]
                for ci, c0 in enumerate(range(0, 3592, 512)):
                    c1 = min(c0 + 512, 3592)
                    st = wst[ci % 2]
                    key = "wst%d" % (ci % 2)
                    k.dma(st[:, :, 0:c1 - c0], w_in[:, c0:c1].rearrange("(k p) n -> p k n", p=128), [], [key])
                    k.cp("gpsimd" if ci % 2 else "vector", Wb[:, :, c0:c1], st[:, :, 0:c1 - c0], [key], ["Wb"])
                S.drain()
                S.emit()

            xs = [SB(P1, "xs%d" % i, [128, D]) for i in range(4)]
            sq = SB(P1, "sq", [128, D])
            ss = SB(P1, "ss", [128, 4])
            rs = SB(P1, "rs", [128, 4])
            xb = [SB(P1, "xb%d" % i, [128, D], BF16) for i in range(4)]
            hT = [SB(P1, "hT%d" % i, [128, 8, 515], BF16) for i in range(2)]
            ktst = [SB(P1, "ktst%d" % i, [128, 512], BF16) for i in range(2)]
            vst = [SB(P1, "vst%d" % i, [128, 512], BF16) for i in range(2)]
            kpre = SB(P1, "kpre", [128, 515])
            cacc = SB(P1, "cacc", [128, 512])
            kT = SB(P1, "kT", [128, 4, 512], BF16)
            qT = SB(P1, "qT", [128, 4, 512], BF16)
            kml = [SB(P1, "kml%d" % i, [128, 512], BF16) for i in range(2)]
            vml = [SB(P1, "vml%d" % i, [128, 512], BF16) for i in range(2)]
            rhsW = [SB(P1, "rhsW%d" % i, [128, 4, 129], BF16) for i in range(2)]
            rhsV = SB(P1, "rhsV", [128, 4, 129], BF16)
            gsb = SB(P1, "gsb", [128, 8])
            nlf = SB(P1, "nlf", [128, 4])
            t4 = SB(P1, "t4", [128, 4, 4])
            t4b = SB(P1, "t4b", [128, 4, 4])
            wv = SB(P1, "wv", [128, 4])
            dec = SB(P1, "dec", [128, 4])
            uu = SB(P1, "uu", [128, 4])
            aa = SB(P1, "aa", [128, 4])
            SuT = SB(P1, "SuT", [128, 128], BF16)
            Sbf = SB(P1, "Sbf", [128, 4, 129], BF16)
            dn = SB(P1, "dn", [128, 2])
            hs = SB(P1, "hs", [128, 512])
            og = SB(P1, "og", [128, 512])
            zz = SB(P1, "zz", [128, 512])
            zq = SB(P1, "zq", [128, 128])
            ssm = SB(P1, "ssm", [128, 4])
            rsm = SB(P1, "rsm", [128, 4])
            ymb = [SB(P1, "ymb%d" % i, [128, 512], BF16) for i in range(2)]
            pT = [PS(P1, "pT%d" % i, [128, 8, 128], BF16) for i in range(2)]
            pP = [PS(P1, "pP%d" % i, [128, 512]) for i in range(2)]
            pG = PS(P1, "pG", [128, 512])
            pS = [PS(P1, "pS%d" % i, [128, 2, 256]) for i in range(2)]
            pO = PS(P1, "pO", [128, 512])
            pOb = pO[:].bitcast(BF16)

            k.memset("vector", hT[0][:, :, 0:3], 0.0, ["hT0"])
            k.memset("vector", hT[1][:, :, 0:3], 0.0, ["hT1"])
            k.memset("vector", rhsV[:], 1.0, ["rhsV"])
            ppi = [0]

            def nextP():
                ppi[0] += 1
                i = ppi[0] % 2
                return pP[i], "pP%d" % i

            LNSC = math.log(128.0 ** -0.5)
            gsbA = SB(P1, "gsbA", [128, 4, 8])
            nlfA = SB(P1, "nlfA", [128, 4, 4])
            wvA = SB(P1, "wvA", [128, 4, 4])
            decA = SB(P1, "decA", [128, 4, 4])
            uuA = SB(P1, "uuA", [128, 4, 4])
            aaA = SB(P1, "aaA", [128, 4, 4])

            def normLoad(ti):
                for b in range(4):
                    gb = ti * 4 + b
                    k.dma(xs[b][:], xseq[gb * 128:(gb + 1) * 128, :], [], ["xs%d" % b])

            def normA(ti):
                for b in range(4):
                    k.act(sq[:], xs[b][:], AF.Square, ["xs%d" % b], ["sq", "ss"], accum_out=ss[:, b:b + 1])
                k.act(rs[:], ss[:], AF.Ln, ["ss"], ["rs"], scale=1.0 / D, bias=1e-6)
                k.act(rs[:], rs[:], AF.Exp, ["rs"], ["rs"], scale=-0.5)
                for b in range(4):
                    k.stt(xb[b][:], xs[b][:], rs[:, b:b + 1], gmix[:], ALU.mult, ALU.mult, ["xs%d" % b, "rs", "gmix"], ["xb%d" % b])

            def normB(ti):
                h_ = hT[ti % 2]
                hk = "hT%d" % (ti % 2)
                hp = hT[(ti + 1) % 2]
                hpk = "hT%d" % ((ti + 1) % 2)
                if ti > 0:
                    k.cp("scalar", h_[:, :, 0:3], hp[:, :, 512:515], [hpk], [hk])
                for b in range(4):
                    pt = pT[b % 2]
                    ptk = "pT%d" % (b % 2)
                    for kc in range(8):
                        k.tr(pt[:, kc, :], xb[b][:, kc * 128:(kc + 1) * 128], identb[:], ["xb%d" % b, "identb"], [ptk])
                    k.cp("scalar", h_[:, :, 3 + b * 128:3 + (b + 1) * 128], pt[:], [ptk], [hk])

            def gatesA(ti):
                h_ = hT[ti % 2]
                hk = "hT%d" % (ti % 2)
                for b in range(4):
                    tcs = slice(3 + b * 128, 3 + (b + 1) * 128)
                    g0 = b * 20
                    for kc in range(8):
                        k.mm(pG[:, g0:g0 + 8], h_[:, kc, tcs], Wb[:, kc, C_G:C_G + 8], ["Wb", hk], ["pG"],
                             start=(kc == 0), stop=(kc == 7))
                for b in range(4):
                    g0 = b * 20
                    k.tt("vector", gsbA[:, b, :], pG[:, g0:g0 + 8], bg[:], ALU.add, ["pG", "bg"], ["gsb%d" % b])
                k.act(nlfA[:], gsbA[:, :, 4:8], AF.Exp, ["gsb0", "gsb1", "gsb2", "gsb3"], ["nlf"], scale=-1.0)
                k.act(nlfA[:], nlfA[:], AF.Ln, ["nlf"], ["nlf"], bias=1.0)

            def gatesB(ti):
                own = ti >= OWN_T0
                for b in range(4):
                    g0 = b * 20
                    nl = nlfA[:, b, :]
                    k.mm(pG[:, g0 + 8:g0 + 12], tri[:], nl, ["tri", "nlf"], ["pG"])
                    k.mm(pG[:, g0 + 12:g0 + 16], ntri[:], nl, ["ntri", "nlf"], ["pG"])
                    k.mm(pG[:, g0 + 16:g0 + 20], ones[:], nl, ["ones", "nlf"], ["pG"])
                for b in range(4):
                    gb = ti * 4 + b
                    g0 = b * 20
                    k.tt("vector", t4[:, b, :], pG[:, g0 + 12:g0 + 16], gsbA[:, b, 0:4], ALU.add, ["pG", "gsb%d" % b], ["t4"])
                    k.act(decA[:, b, :], pG[:, g0 + 16:g0 + 20], AF.Exp, ["pG"], ["dec%d" % b], scale=-1.0)
                    if own:
                        k.tt("vector", t4b[:, b, :], pG[:, g0 + 8:g0 + 12], gsbA[:, b, 0:4], ALU.add, ["pG", "gsb%d" % b], ["t4b"])
                        k.act(aaA[:, b, :], pG[:, g0 + 8:g0 + 12], AF.Exp, ["pG"], ["aa%d" % b], scale=-1.0, bias=LNSC)
                k.act(t4[:], t4[:], AF.Exp, ["t4"], ["t4"])
                for b in range(4):
                    gb = ti * 4 + b
                    k.ts("vector", wvA[:, b, :], t4[:, b, :], vld[:, gb:gb + 1], None, ALU.mult, None, ["t4", "vld"], ["wv%d" % b])
                if own:
                    k.act(uuA[:], t4b[:], AF.Exp, ["t4b"], ["uu0", "uu1", "uu2", "uu3"])

            def projK(ti):
                h_ = hT[ti % 2]
                hk = "hT%d" % (ti % 2)
                for h in range(4):
                    pp, ppk = nextP()
                    for kc in range(8):
                        k.mm(pp[:], Wb[:, kc, C_KDA + h * 128:C_KDA + (h + 1) * 128], h_[:, kc, 3:515],
                             ["Wb", hk], [ppk], start=(kc == 0), stop=(kc == 7))
                    st = ktst[h % 2]
                    stk = "ktst%d" % (h % 2)
                    k.cp("vector", st[:], pp[:], [ppk], [stk])
                    k.dma(KTscr[h, :, ti * 512:(ti + 1) * 512], st[:], [stk], ["KTscr"])

            def projQ(ti):
                h_ = hT[ti % 2]
                hk = "hT%d" % (ti % 2)
                qc0 = (ti - OWN_T0) * 512
                for h in range(4):
                    for c in range(2):
                        pp, ppk = nextP()
                        col = C_QDA + h * 128 + c * 64
                        for kc in range(8):
                            k.mm(pp[0:64, :], Wb[:, kc, col:col + 64], h_[:, kc, 3:515], ["Wb", hk], [ppk],
                                 start=(kc == 0), stop=(kc == 7))
                        QT = QT0 if c == 0 else QT1
                        k.act(QT[0:64, h, qc0:qc0 + 512], pp[0:64, :], AF.Copy, [ppk], ["QT%d" % c], scale=0.125)

            def projConv(ti):
                own = ti >= OWN_T0
                h_ = hT[ti % 2]
                hk = "hT%d" % (ti % 2)
                for which in ([1, 0] if own else [1]):
                    cbase = C_MLK if which == 1 else C_MLQ
                    dst = kT if which == 1 else qT
                    dstk = "kT" if which == 1 else "qT"
                    for c in range(4):
                        pp, ppk = nextP()
                        for kc in range(8):
                            k.mm(pp[:], Wb[:, kc, cbase + c * 128:cbase + (c + 1) * 128], h_[:, kc, 3:515],
                                 ["Wb", hk], [ppk], start=(kc == 0), stop=(kc == 7))
                        for kc in range(8):
                            k.mm(pG[:, 96:99], Wb[:, kc, cbase + c * 128:cbase + (c + 1) * 128], h_[:, kc, 0:3],
                                 ["Wb", hk], ["pG"], start=(kc == 0), stop=(kc == 7))
                        k.cp("scalar", kpre[:, 3:515], pp[:], [ppk], ["kpre"])
                        k.cp("scalar", kpre[:, 0:3], pG[:, 96:99], ["pG"], ["kpre"])
                        cc = which * 4 + c
                        k.ts("vector", cacc[:], kpre[:, 0:512], cw[:, cc, 0:1], cb[:, cc:cc + 1], ALU.mult, ALU.add,
                             ["kpre", "cw", "cb"], ["cacc"])
                        for j in range(1, 4):
                            k.stt(cacc[:], kpre[:, j:j + 512], cw[:, cc, j:j + 1], cacc[:], ALU.mult, ALU.add,
                                  ["kpre", "cw", "cacc"], ["cacc"])
                        k.act(dst[:, c, :], cacc[:], AF.Silu, ["cacc"], [dstk])

            def blocks(ti):
                own = ti >= OWN_T0
                h_ = hT[ti % 2]
                hk = "hT%d" % (ti % 2)
                for b in range(4):
                    gb = ti * 4 + b
                    tcs = slice(3 + b * 128, 3 + (b + 1) * 128)
                    wv, wvk = wvA[:, b, :], "wv%d" % b
                    dec, deck = decA[:, b, :], "dec%d" % b
                    uu, uuk = uuA[:, b, :], "uu%d" % b
                    aa, aak = aaA[:, b, :], "aa%d" % b
                    pp, ppk = nextP()
                    for kc in range(8):
                        k.mm(pp[:], h_[:, kc, tcs], Wb[:, kc, C_MLV:C_MLV + 512], ["Wb", hk], [ppk],
                             start=(kc == 0), stop=(kc == 7))
                    vm = vml[gb % 2]
                    vmk = "vml%d" % (gb % 2)
                    k.cp("scalar", vm[:], pp[:], [ppk], [vmk])
                    for h in range(4):
                        k.tr(pOb[:, h * 128:(h + 1) * 128], kT[:, h, b * 128:(b + 1) * 128], identb[:],
                             ["kT", "identb"], ["pO"])
                    km = kml[gb % 2]
                    kmk = "kml%d" % (gb % 2)
                    k.cp("vector", km[:], pOb[:, 0:512], ["pO"], [kmk])
                    rw = rhsW[gb % 2]
                    rwk = "rhsW%d" % (gb % 2)
                    for h in range(4):
                        k.act(rw[:, h, 0:128], vm[:, h * 128:(h + 1) * 128], AF.Copy, [vmk, wvk], [rwk], scale=wv[:, h:h + 1])
                    k.cp("scalar", rw[:, :, 128:129], wv.unsqueeze(2), [wvk], [rwk])
                    pp, ppk = nextP()
                    for kc in range(8):
                        k.mm(pp[:], h_[:, kc, tcs], Wb[:, kc, C_VDA:C_VDA + 512], ["Wb", hk], [ppk],
                             start=(kc == 0), stop=(kc == 7))
                    st = vst[gb % 2]
                    stk = "vst%d" % (gb % 2)
                    k.cp("vector", st[:], pp[:], [ppk], [stk])
                    k.dma(Vscr[:, :, gb, :].rearrange("h p d -> p h d"), st[:].rearrange("p (h d) -> p h d", h=4),
                          [stk], ["Vscr"])
                    if own:
                        qb = gb - OWN_BLK0
                        k.cp("scalar", rhsV[:, :, 0:128], vm[:].rearrange("p (h d) -> p h d", h=4), [vmk], ["rhsV"])
                        k.cp("scalar", Sbf[:], State[:], ["State"], ["Sbf"])
                        pp, ppk = nextP()
                        for kc in range(8):
                            k.mm(pp[:], h_[:, kc, tcs], Wb[:, kc, C_MLO:C_MLO + 512], ["Wb", hk], [ppk],
                                 start=(kc == 0), stop=(kc == 7))
                        k.act(og[:], pp[:], AF.Sigmoid, [ppk], ["og"])
                        for h in range(4):
                            bs = slice(b * 128, (b + 1) * 128)
                            k.mm(pO[:, 0:128], kT[:, h, bs], qT[:, h, bs], ["kT", "qT"], ["pO"])
                            k.stt(SuT[:], pO[:, 0:128], uu[:, h:h + 1], tri[:], ALU.mult, ALU.mult,
                                  ["pO", uuk, "tri"], ["SuT"])
                            k.mm(pO[:, 256:385], SuT[:], rhsV[:, h, :], ["SuT", "rhsV"], ["pO"], start=True, stop=False)
                            k.mm(pO[:, 256:385], qT[:, h, bs], Sbf[:, h, :], ["qT", "Sbf"], ["pO"], start=False, stop=True)
                            k.tt("vector", dn[:, 0:1], pO[:, 384:385], aa[:, h:h + 1], ALU.mult, ["pO", aak], ["dn"])
                            k.ts("vector", dn[:, 1:2], dn[:, 0:1], -1.0, None, ALU.mult, None, ["dn"], ["dn"])
                            k.tt("vector", dn[:, 0:1], dn[:, 0:1], dn[:, 1:2], ALU.max, ["dn"], ["dn"])
                            k.ts("vector", dn[:, 0:1], dn[:, 0:1], 1.0, None, ALU.max, None, ["dn"], ["dn"])
                            S.op("vector", lambda e: e.reciprocal(out=dn[:, 0:1], in_=dn[:, 0:1]), ["dn"], ["dn"])
                            k.tt("vector", dn[:, 1:2], dn[:, 0:1], aa[:, h:h + 1], ALU.mult, ["dn", aak], ["dn"])
                            k.ts("vector", hs[:, h * 128:(h + 1) * 128], pO[:, 256:384], dn[:, 1:2], None, ALU.mult, None,
                                 ["pO", "dn"], ["hs"])
                        k.tt("vector", zz[:], og[:], hs[:], ALU.mult, ["og", "hs"], ["zz"])
                        for h in range(4):
                            k.act(zq[:], zz[:, h * 128:(h + 1) * 128], AF.Square, ["zz"], ["zq", "ssm"],
                                  accum_out=ssm[:, h:h + 1])
                        k.act(rsm[:], ssm[:], AF.Ln, ["ssm"], ["rsm"], scale=1.0 / 128, bias=1e-6)
                        k.act(rsm[:], rsm[:], AF.Exp, ["rsm"], ["rsm"], scale=-0.5)
                        ym = ymb[qb % 2]
                        ymk = "ymb%d" % (qb % 2)
                        for h in range(4):
                            hsl = slice(h * 128, (h + 1) * 128)
                            k.stt(ym[:, hsl], zz[:, hsl], rsm[:, h:h + 1], gml[:, hsl], ALU.mult, ALU.mult,
                                  ["zz", "rsm", "gml"], [ymk])
                        k.dma(Yscr[qb * 128:(qb + 1) * 128, 512:1024], ym[:], [ymk], ["Yscr"])
                    for h in range(4):
                        ps = pS[h // 2]
                        psk = "pS%d" % (h // 2)
                        k.mm(ps[:, h % 2, 0:129], km[:, h * 128:(h + 1) * 128], rw[:, h, :], [kmk, rwk], [psk])
                    for h in range(4):
                        ps = pS[h // 2]
                        psk = "pS%d" % (h // 2)
                        k.stt(State[:, h, :], State[:, h, :], dec[:, h:h + 1], ps[:, h % 2, 0:129], ALU.mult, ALU.add,
                              ["State", deck, psk], ["State"])

            normLoad(0)
            normA(0)
            normB(0)
            for ti in range(NT):
                own = ti >= OWN_T0
                if ti + 1 < NT:
                    normLoad(ti + 1)
                gatesA(ti)
                projConv(ti)
                projK(ti)
                gatesB(ti)
                if ti + 1 < NT:
                    normA(ti + 1)
                if own:
                    projQ(ti)
                if ti + 1 < NT:
                    normB(ti + 1)
                blocks(ti)
            S.drain()
            S.emit()
        with ExitStack() as P2:
            KT0 = SB(P2, "KT0", [67, T_ALL], BF16)
            KT1 = SB(P2, "KT1", [67, T_ALL], BF16)
            Vh = SB(P2, "Vh", [128, NBLK, 129], BF16)
            btab = SB(P2, "btab", [128, 4, 4, NBLK])
            dmask = SB(P2, "dmask", [128, 4, 512])
            gsub2 = SB(P2, "gsub2", [128, 128])
            Pm = [SB(P2, "P%d" % i, [128, 2, 512], BF16) for i in range(3)]
            Sm = SB(P2, "Sm", [128, 2, 512])
            rl = SB(P2, "rl", [128, 2])
            od = SB(P2, "od", [128, 128])
            oq = SB(P2, "oq", [128, 128])
            sso = SB(P2, "sso", [128, 1])
            yda = [SB(P2, "yda%d" % i, [128, 128], BF16) for i in range(2)]
            pSm = [PS(P2, "pSm%d" % i, [128, 2, 512]) for i in range(2)]
            pOa = [PS(P2, "pOa%d" % i, [128, 512]) for i in range(3)]
            k.dma(btab[:], c_bias, [], ["btab"])
            k.dma(dmask[:], c_dmask, [], ["dmask"])
            k.dma(gsub2[:], da_subln_g.partition_broadcast(128), [], ["gsub2"])
            k.memset("vector", KT0[64:66, :], 1.0, ["KT0"])
            k.memset("vector", KT1[64:66, :], 1.0, ["KT1"])
            k.dma(KT0[66:67, :], padrow, [], ["KT0"])
            k.dma(KT1[66:67, :], padrow, [], ["KT1"])
            k.memset("vector", Vh[:, :, 128:129], 1.0, ["Vh"])
            it = 0
            for h in range(4):
                k.dma(KT0[0:64, :], KTscr[h, 0:64, :], ["KTscr"], ["KT0"])
                k.dma(KT1[0:64, :], KTscr[h, 64:128, :], ["KTscr"], ["KT1"])
                k.dma(Vh[:, :, 0:128], Vscr[h], ["Vscr"], ["Vh"])
                for g in range(4):
                    nkb = OWN_BLK0 + 4 * g + 4
                    started = [False, False, False]
                    def qk(kb, i):
                        for m in range(2):
                            KT = KT0 if m == 0 else KT1
                            QT = QT0 if m == 0 else QT1
                            k.mm(pSm[i][:, m, :], KT[0:67, kb * 128:(kb + 1) * 128], QT[0:67, h, g * 512:(g + 1) * 512],
                                 ["KT%d" % m, "QT%d" % m], ["pSm%d" % i])

                    def pv(kb):
                        pi = kb % 3
                        for qb in range(4):
                            for m in range(2):
                                e = qb * 2 + m
                                bank, off = e // 3, (e % 3) * 129
                                st_ = not started[bank]
                                started[bank] = True
                                k.mm(pOa[bank][:, off:off + 129], Pm[pi][:, m, qb * 128:(qb + 1) * 128], Vh[:, kb, :],
                                     ["P%d" % pi, "Vh"], ["pOa%d" % bank], start=st_, stop=(kb == nkb - 1))

                    qk(0, it % 2)
                    for kb in range(nkb):
                        diag = kb >= OWN_BLK0 + 4 * g
                        i = it % 2
                        it += 1
                        pi = kb % 3
                        src, srck = pSm[i][:], "pSm%d" % i
                        if diag:
                            dm_ = dmask[:, kb - (OWN_BLK0 + 4 * g), :]
                            k.tt("vector", Sm[:], src, dm_.unsqueeze(1).broadcast_to([128, 2, 512]), ALU.add,
                                 [srck, "dmask"], ["Sm"])
                            src, srck = Sm[:], "Sm"
                        k.act(Pm[pi][:], src, AF.Exp, [srck, "btab"], ["P%d" % pi], bias=btab[:, h, g, kb:kb + 1])
                        if kb + 1 < nkb:
                            qk(kb + 1, it % 2)
                        if kb >= 1:
                            pv(kb - 1)
                    pv(nkb - 1)
                    for qb in range(4):
                        e0, e1 = qb * 2, qb * 2 + 1
                        b0, o0 = e0 // 3, (e0 % 3) * 129
                        b1, o1 = e1 // 3, (e1 % 3) * 129
                        S.op("vector", lambda e, o=rl[:, 0:1], i_=pOa[b0][:, o0 + 128:o0 + 129]: e.reciprocal(out=o, in_=i_),
                             ["pOa%d" % b0], ["rl"])
                        S.op("vector", lambda e, o=rl[:, 1:2], i_=pOa[b1][:, o1 + 128:o1 + 129]: e.reciprocal(out=o, in_=i_),
                             ["pOa%d" % b1], ["rl"])
                        k.tt("vector", rl[:, 1:2], rl[:, 1:2], lamc[:], ALU.mult, ["rl", "lamc"], ["rl"])
                        k.ts("vector", od[:], pOa[b0][:, o0:o0 + 128], rl[:, 0:1], None, ALU.mult, None,
                             ["pOa%d" % b0, "rl"], ["od"])
                        k.stt(od[:], pOa[b1][:, o1:o1 + 128], rl[:, 1:2], od[:], ALU.mult, ALU.add,
                              ["pOa%d" % b1, "rl", "od"], ["od"])
                        k.act(oq[:], od[:], AF.Square, ["od"], ["oq", "sso"], accum_out=sso[:])
                        k.act(sso[:], sso[:], AF.Ln, ["sso"], ["sso"], scale=1.0 / 128, bias=1e-6)
                        k.act(sso[:], sso[:], AF.Exp, ["sso"], ["sso"], scale=-0.5)
                        k.ts("vector", sso[:], sso[:], 1.0 - LAMBDA_INIT, None, ALU.mult, None, ["sso"], ["sso"])
                        qg = g * 4 + qb
                        yd = yda[qg % 2]
                        ydk = "yda%d" % (qg % 2)
                        k.stt(yd[:], od[:], sso[:], gsub2[:], ALU.mult, ALU.mult, ["od", "sso", "gsub2"], [ydk])
                        k.dma(Yscr[qg * 128:(qg + 1) * 128, h * 128:(h + 1) * 128], yd[:], [ydk], ["Yscr"])
            S.drain()
            S.emit()

        P12.close()
        if stage == 2:
            with ExitStack() as PD:
                yb = SB(PD, "dbg_yb", [128, NQB, D], BF16)
                yf = SB(PD, "dbg_yf", [128, NQB, D])
                k.dma(yb[:], Yscr.rearrange("(q p) d -> p q d", p=128), ["Yscr"], ["dbg_yb"])
                k.cp("vector", yf[:], yb[:], ["dbg_yb"], ["dbg_yf"])
                k.dma(out.rearrange("(q p) d -> p q d", p=128), yf[:], ["dbg_yf"], ["out"])
                S.final_wait("sync", ["out"])
                S.emit()
            return nc

        own_row0 = T_ALL - T_OWN

        def rmsnorm_bf(ctx_ss, ctx_rs, junk, junkk, src, srck, gB, gk, dst, dstk):
            k.act(junk, src, AF.Square, [srck], [junkk, "nss"], accum_out=ctx_ss[:])
            k.act(ctx_rs[:], ctx_ss[:], AF.Ln, ["nss"], ["nrs"], scale=1.0 / D, bias=1e-6)
            k.act(ctx_rs[:], ctx_rs[:], AF.Exp, ["nrs"], ["nrs"], scale=-0.5)
            k.stt(dst, src, ctx_rs[:], gB, ALU.mult, ALU.mult, [srck, "nrs", gk], [dstk])

        PW = ExitStack()
        Wsh = SB(PW, "Wsh", [128, 8, 2048], BF16)
        Wple = SB(PW, "Wple", [128, 10, 1024], BF16)
        subkT = SB(PW, "subkT", [128, 16, 128], BF16)
        with ExitStack() as P3:
            Wo = SB(P3, "Wo", [128, 8, D], BF16)
            yb = [SB(P3, "yb%d" % i, [128, D], BF16) for i in range(2)]
            yT = [SB(P3, "yT%d" % i, [128, 8, 128], BF16) for i in range(2)]
            xr = [SB(P3, "xr%d" % i, [128, D]) for i in range(2)]
            x1 = [SB(P3, "x1_%d" % i, [128, D]) for i in range(2)]
            pT3 = [PS(P3, "pT3_%d" % i, [128, 8, 128], BF16) for i in range(2)]
            pY = [PS(P3, "pY%d" % i, [128, 2, 512]) for i in range(2)]
            for c0 in range(0, D, 512):
                k.dma(Wo[:, :, c0:c0 + 512], w_out[:, c0:c0 + 512].rearrange("(k p) n -> p k n", p=128), [], ["Wo"], eng="gpsimd")
            skf3 = SB(P3, "skf3", [128, 128])
            skb3 = SB(P3, "skb3", [128, 128], BF16)
            for hc in range(16):
                k.dma(skf3[:], sub_keys[hc], [], ["skf3"])
                k.cp("vector", skb3[:], skf3[:], ["skf3"], ["skb3"])
                k.tr(pT3[0][:, 0, :], skb3[:], identb[:], ["skb3", "identb"], ["pT3_0"])
                k.cp("scalar", subkT[:, hc, :], pT3[0][:, 0, :], ["pT3_0"], ["subkT"])
            for c0 in range(0, 2048, 512):
                k.dma(Wsh[:, :, c0:c0 + 512], w_query[:, c0:c0 + 512].rearrange("(k p) n -> p k n", p=128), [], ["Wsh"],
                      eng="gpsimd")
            for c0 in range(0, 1024, 512):
                k.dma(Wple[:, 0:8, c0:c0 + 512], w_ple_gate[:, c0:c0 + 512].rearrange("(k p) n -> p k n", p=128), [], ["Wple"],
                      eng="gpsimd")
                k.dma(Wple[:, 8:10, c0:c0 + 512], w_ple_proj[:, c0:c0 + 512].rearrange("(k p) n -> p k n", p=128), [], ["Wple"],
                      eng="gpsimd")
            for qb in range(NQB):
                i = qb % 2
                k.dma(yb[i][:], Yscr[qb * 128:(qb + 1) * 128, :], ["Yscr"], ["yb%d" % i])
                k.dma(xr[i][:], xseq[own_row0 + qb * 128:own_row0 + (qb + 1) * 128, :], [], ["xr%d" % i])
                for kc in range(8):
                    k.tr(pT3[i][:, kc, :], yb[i][:, kc * 128:(kc + 1) * 128], identb[:], ["yb%d" % i, "identb"], ["pT3_%d" % i])
                k.cp("scalar", yT[i][:], pT3[i][:], ["pT3_%d" % i], ["yT%d" % i])
                for half in range(2):
                    for kc in range(8):
                        k.mm(pY[i][:, half, :], yT[i][:, kc, :], Wo[:, kc, half * 512:(half + 1) * 512],
                             ["yT%d" % i, "Wo"], ["pY%d" % i], start=(kc == 0), stop=(kc == 7))
                k.tt("vector", x1[i][:], pY[i][:].rearrange("p a b -> p (a b)"), xr[i][:], ALU.add,
                     ["pY%d" % i, "xr%d" % i], ["x1_%d" % i])
                k.dma(X1scr[qb * 128:(qb + 1) * 128, :], x1[i][:], ["x1_%d" % i], ["X1scr"])
            S.drain()
            S.emit()

        if stage == 3:
            with ExitStack() as PD:
                yf = SB(PD, "dbg_yf", [128, NQB, D])
                k.dma(yf[:], X1scr.rearrange("(q p) d -> p q d", p=128), ["X1scr"], ["dbg_yf"])
                k.dma(out.rearrange("(q p) d -> p q d", p=128), yf[:], ["dbg_yf"], ["out"])
                S.final_wait("sync", ["out"])
                S.emit()
            return nc

        with ExitStack() as P4:
            gffn = SB(P4, "gffn", [128, D])
            s1t = [SB(P4, "s1t%d" % i, [128, 128]) for i in range(2)]
            wt = SB(P4, "wt", [128, 4, 8])
            E0 = SB(P4, "E0", [128, 4, 8, 128])
            E1 = SB(P4, "E1", [128, 4, 8, 128])
            acc = SB(P4, "acc", [128, 4, D])
            xnT = SB(P4, "xnT", [128, 8, 512], BF16)
            Uf = [SB(P4, "Uf%d" % i, [128, 2, D]) for i in range(2)]
            Vf = [SB(P4, "Vf0", [128, 2, D]), SB(P4, "Vf1", [128, 1, D])]
            Ub = [SB(P4, "Ub%d" % i, [128, 2, D], BF16) for i in range(2)]
            Vb = [SB(P4, "Vb%d" % i, [128, 2, D], BF16) for i in range(3)]
            UT = [SB(P4, "UT%d" % i, [128, 8, 128], BF16) for i in range(2)]
            gA = [[SB(P4, "gA%d_%d" % (i, j), [128, 512], BF16) for j in range(2)] for i in range(2)]
            GA = [[SB(P4, "GA%d_%d" % (i, j), [128, 512], BF16) for j in range(2)] for i in range(2)]
            NW, NT2 = 6, 12
            Wp = [SB(P4, "Wp%d" % i, [128, 2, 128]) for i in range(NW)]
            T2 = [SB(P4, "T2_%d" % i, [128, 2, 128], BF16) for i in range(NT2)]
            nss = SB(P4, "nss", [128, 1])
            nrs = SB(P4, "nrs", [128, 1])
            xnb = SB(P4, "xnb", [128, D], BF16)
            qTs = [SB(P4, "qTs%d" % i, [128, 512], BF16) for i in range(2)]
            s0t = [SB(P4, "s0t%d" % i, [128, 128]) for i in range(2)]
            tmp = [SB(P4, "tmpk%d" % i, [128, 256]) for i in range(2)]
            v16 = [SB(P4, "v16_%d" % i, [128, 2, 16]) for i in range(2)]
            cand = [SB(P4, "cand%d" % i, [128, 256]) for i in range(2)]
            t16 = [SB(P4, "t16_%d" % i, [128, 16]) for i in range(2)]
            sm = [SB(P4, "sm%d" % i, [128, 8]) for i in range(2)]
            skf = SB(P4, "skf", [128, 128])
            skb = SB(P4, "skb", [128, 128], BF16)
            pf = SB(P4, "pf", [128, 256])
            pb = SB(P4, "pb", [128, 256], BF16)
            pTt = SB(P4, "pTt", [128, 2, 128], BF16)
            pQ = PS(P4, "pQ", [128, 512])
            pU = pQ[:].bitcast(BF16).rearrange("p (k e) -> p k e", k=8)
            pA = PS(P4, "pA", [128, 512])
            pGt = PS(P4, "pGt", [128, 4, 512])
            pV = PS(P4, "pV", [128, 2, 512])
            k.dma(gffn[:], norm_ffn_g.partition_broadcast(128), [], ["gffn"])
            for qt in range(4):
                for blk in range(4):
                    r0 = (qt * 4 + blk) * 128
                    k.dma(acc[:, blk, :], X1scr[r0:r0 + 128, :], ["X1scr"], ["acc"])
                    rmsnorm_bf(nss, nrs, Vf[1][:, 0, :], "Vf1", acc[:, blk, :], "acc", gffn[:], "gffn", xnb[:], "xnb")
                    for kc in range(8):
                        k.tr(pU[:, kc, :], xnb[:, kc * 128:(kc + 1) * 128], identb[:], ["xnb", "identb"], ["pU"])
                    k.cp("scalar", xnT[:, :, blk * 128:(blk + 1) * 128], pU, ["pU"], ["xnT"])
                for h in range(8):
                    for c in range(2):
                        hc = h * 2 + c
                        for kc in range(8):
                            k.mm(pQ[:], Wsh[:, kc, hc * 128:(hc + 1) * 128], xnT[:, kc, :], ["Wsh", "xnT"], ["pU"],
                                 start=(kc == 0), stop=(kc == 7))
                        k.cp("scalar", qTs[c][:], pQ[:], ["pU"], ["qTs%d" % c])
                    def chain_ops(blk, h, p):
                        bs = slice(blk * 128, (blk + 1) * 128)
                        P = str(p)
                        s0_, s1_, v_, c_, t_, tm_, sm_ = s0t[p], s1t[p], v16[p], cand[p], t16[p], tmp[p], sm[p]
                        o = []
                        o.append(lambda: k.mm(pA[:, p * 256:p * 256 + 128], qTs[0][:, bs], subkT[:, h * 2, :], ["qTs0", "subkT"], ["pA"]))
                        o.append(lambda: k.mm(pA[:, p * 256 + 128:p * 256 + 256], qTs[1][:, bs], subkT[:, h * 2 + 1, :], ["qTs1", "subkT"], ["pA"]))
                        o.append(lambda: k.cp("vector", s0_[:], pA[:, p * 256:p * 256 + 128], ["pA"], ["s0t" + P]))
                        o.append(lambda: k.cp("vector", s1_[:], pA[:, p * 256 + 128:p * 256 + 256], ["pA"], ["s1t" + P]))
                        for c, src, sk in ((0, s0_, "s0t" + P), (1, s1_, "s1t" + P)):
                            o.append(lambda c=c, src=src, sk=sk: S.op("vector", lambda e: e.max(out=v_[:, c, 0:8], in_=src[:]), [sk], ["v16" + P]))
                            o.append(lambda c=c, src=src, sk=sk: S.op("vector", lambda e: e.match_replace(
                                out=tm_[:, 0:128], in_to_replace=v_[:, c, 0:8], in_values=src[:], imm_value=-1e30), ["v16" + P, sk], ["tmpk" + P]))
                            o.append(lambda c=c: S.op("vector", lambda e: e.max(out=v_[:, c, 8:16], in_=tm_[:, 0:128]), ["tmpk" + P], ["v16" + P]))
                        o.append(lambda: k.tt("vector", c_[:].rearrange("p (a b) -> p a b", a=16),
                                              v_[:, 0, :].unsqueeze(2).broadcast_to([128, 16, 16]),
                                              v_[:, 1, :].unsqueeze(1).broadcast_to([128, 16, 16]), ALU.add, ["v16" + P], ["cand" + P]))
                        o.append(lambda: S.op("vector", lambda e: e.max(out=t_[:, 0:8], in_=c_[:]), ["cand" + P], ["t16" + P]))
                        o.append(lambda: S.op("vector", lambda e: e.match_replace(out=tm_[:], in_to_replace=t_[:, 0:8], in_values=c_[:],
                                                                                  imm_value=-1e30), ["t16" + P, "cand" + P], ["tmpk" + P]))
                        o.append(lambda: S.op("vector", lambda e: e.max(out=t_[:, 8:16], in_=tm_[:]), ["tmpk" + P], ["t16" + P]))
                        o.append(lambda: k.ts("vector", sm_[:, 0:1], t_[:, 0:1], -1.0, None, ALU.mult, None, ["t16" + P], ["sm" + P]))
                        o.append(lambda: k.ts("vector", sm_[:, 2:4], v_[:, :, 0], -1.0, None, ALU.mult, None, ["v16" + P], ["sm" + P]))
                        o.append(lambda: k.act(c_[:, 0:16], t_[:], AF.Exp, ["t16" + P, "sm" + P], ["cand" + P, "smz" + P],
                                               bias=sm_[:, 0:1], accum_out=sm_[:, 4:5]))
                        o.append(lambda: S.op("vector", lambda e: e.reciprocal(out=sm_[:, 5:6], in_=sm_[:, 4:5]), ["smz" + P], ["smr" + P]))
                        o.append(lambda: k.ts("vector", wt[:, blk, h:h + 1], c_[:, 15:16], sm_[:, 5:6], 0.9997, ALU.mult, ALU.mult,
                                              ["cand" + P, "smr" + P], ["wt"]))
                        o.append(lambda: k.act(E0[:, blk, h, :], s0_[:], AF.Exp, ["s0t" + P, "sm" + P], ["E0"], bias=sm_[:, 2:3]))
                        o.append(lambda: k.ts("vector", E0[:, blk, h, :], E0[:, blk, h, :], sm_[:, 5:6], None, ALU.mult, None,
                                              ["E0", "smr" + P], ["E0"]))
                        o.append(lambda: k.act(E1[:, blk, h, :], s1_[:], AF.Exp, ["s1t" + P, "sm" + P], ["E1"], bias=sm_[:, 3:4]))
                        return o

                    for bp in range(2):
                        A_ = chain_ops(2 * bp, h, 0)
                        B_ = chain_ops(2 * bp + 1, h, 1)
                        for a_, b_ in zip(A_, B_):
                            a_()
                            b_()
                def eload(sc, whichs):
                    rows = slice(sc * 256, (sc + 1) * 256)
                    if "u" in whichs:
                        k.dma(Ub[sc % 2][:], expert_u[rows, :].rearrange("(s p) d -> p s d", p=128), [], ["Ub%d" % (sc % 2)],
                              eng="gpsimd")
                    if "v" in whichs:
                        k.dma(Vb[sc % 3][:], expert_v[rows, :].rearrange("(s p) d -> p s d", p=128), [], ["Vb%d" % (sc % 3)],
                              eng="gpsimd")

                def pro_steps(sc):
                    sl = sc % 2
                    steps = []
                    for sub in range(2):
                        def s_tr(sub=sub):
                            for kc in range(8):
                                k.tr(pU[:, kc, :], Ub[sl][:, sub, kc * 128:(kc + 1) * 128], identb[:], ["Ub%d" % sl, "identb"], ["pU"])
                        def s_cp(sub=sub):
                            k.cp("scalar", UT[sub][:], pU, ["pU"], ["UT%d" % sub])
                        def s_mmA(sub=sub):
                            for kc in range(0, 4):
                                k.mm(pA[:], UT[sub][:, kc, :], xnT[:, kc, :], ["UT%d" % sub, "xnT"], ["pA"],
                                     start=(kc == 0), stop=(kc == 7))
                        def s_mmB(sub=sub):
                            for kc in range(4, 8):
                                k.mm(pA[:], UT[sub][:, kc, :], xnT[:, kc, :], ["UT%d" % sub, "xnT"], ["pA"],
                                     start=(kc == 0), stop=(kc == 7))
                        def s_ge(sub=sub):
                            k.act(gA[sl][sub][:], pA[:], AF.Gelu, ["pA"], ["gA%d_%d" % (sl, sub)])
                        steps += [s_tr, s_cp, s_mmA, s_mmB, s_ge]
                    return steps

                eload(0, "uv")
                eload(1, "u")
                for st in pro_steps(0):
                    st()
                def epi_steps(sc):
                    sl3 = sc % 3
                    gp = sc % 2
                    steps = []
                    for blk in range(4):
                        def e_mm(half, blk=blk):
                            for sub in range(2):
                                k.mm(pV[:, half, :], GA[gp][sub][:, blk * 128:(blk + 1) * 128],
                                     Vb[sl3][:, sub, half * 512:(half + 1) * 512],
                                     ["GA%d_%d" % (gp, sub), "Vb%d" % sl3], ["pV"], start=(sub == 0), stop=(sub == 1))
                        def e_add(blk=blk):
                            k.tt("vector", acc[:, blk, :], pV[:].rearrange("p a b -> p (a b)"), acc[:, blk, :], ALU.add,
                                 ["pV", "acc"], ["acc"])
                        steps += [lambda e_mm=e_mm: e_mm(0), lambda e_mm=e_mm: e_mm(1), e_add]
                    return steps

                def ga_mult(sc, sub):
                    gp = sc % 2
                    k.tt("vector", GA[gp][sub][:], pGt[:, gp * 2 + sub, :], gA[gp][sub][:], ALU.mult,
                         ["pGt%d" % (gp * 2 + sub), "gA%d_%d" % (gp, sub)], ["GA%d_%d" % (gp, sub)])

                ui = 0
                wi = 0
                for sc in range(64):
                    if sc + 2 < 64:
                        eload(sc + 2, "u")
                    if sc + 1 < 64:
                        eload(sc + 1, "v")
                    pst = pro_steps(sc + 1) if sc + 1 < 64 else [lambda: None] * 10
                    est = epi_steps(sc - 1) if sc > 0 else [lambda: None] * 12
                    sched_ = {0: [pst[0]], 8: [pst[1]], 10: [est[0]], 12: [est[1]], 14: [pst[2]], 16: [est[2], pst[5]],
                              18: [pst[3]], 20: [est[3]], 22: [est[4]], 24: [pst[6]], 26: [est[5]], 28: [pst[4]],
                              30: [est[6]], 32: [est[7]], 34: [pst[7]], 36: [est[8]], 38: [pst[8]], 40: [est[9]],
                              42: [est[10]], 46: [est[11]], 50: [pst[9]]}
                    if sc > 0:
                        sched_[2] = [lambda sc=sc: ga_mult(sc - 1, 0)]
                        sched_[4] = [lambda sc=sc: ga_mult(sc - 1, 1)]
                    gp = sc % 2
                    un = 0
                    for blk in range(4):
                        for h in range(8):
                            for u_ in (un, un + 1):
                                for f_ in sched_.get(u_, ()):
                                    f_()
                            un += 2
                            j, j2 = ui % NW, ui % NT2
                            ui += 1
                            for sub in range(2):
                                n1 = sc * 2 + sub
                                wi += 1
                                if wi % 4 == 0:
                                    k.ts("vector", Wp[j][:, sub, :], E1[:, blk, h, :], E0[:, blk, h, n1:n1 + 1], None, ALU.mult, None,
                                         ["E1", "E0"], ["Wp%d" % j])
                                else:
                                    k.act(Wp[j][:, sub, :], E1[:, blk, h, :], AF.Copy, ["E1", "E0"], ["Wp%d" % j],
                                          scale=E0[:, blk, h, n1:n1 + 1])
                            k.stt(T2[j2][:], Wp[j][:], wt[:, blk, h:h + 1], Wp[j][:], ALU.is_ge, ALU.mult,
                                  ["Wp%d" % j, "wt"], ["T2_%d" % j2])
                            for sub in range(2):
                                k.mm(pGt[:, gp * 2 + sub, blk * 128:(blk + 1) * 128], T2[j2][:, sub, :], identb[:],
                                     ["T2_%d" % j2, "identb"], ["pGt%d" % (gp * 2 + sub)], start=(h == 0), stop=(h == 7))
                ga_mult(63, 0)
                ga_mult(63, 1)
                for f_ in epi_steps(63):
                    f_()
                gt, x3 = Uf[0][:, 0, :], Uf[0][:, 1, :]
                gple, gfin = Uf[1][:, 0, :], Uf[1][:, 1, :]
                k.dma(gple, norm_ple_g.partition_broadcast(128), [], ["Uf1"])
                k.dma(gfin, final_norm_g.partition_broadcast(128), [], ["Uf1"])
                for blk in range(4):
                    r0 = (qt * 4 + blk) * 128
                    rmsnorm_bf(nss, nrs, Vf[1][:, 0, :], "Vf1", acc[:, blk, :], "acc", gple, "Uf1", xnb[:], "xnb")
                    for kc in range(8):
                        k.tr(pU[:, kc, :], xnb[:, kc * 128:(kc + 1) * 128], identb[:], ["xnb", "identb"], ["pU"])
                    k.cp("scalar", UT[0][:], pU, ["pU"], ["UT0"])
                    for half in range(2):
                        for kc in range(8):
                            k.mm(pV[:, half, :], UT[0][:, kc, :], Wple[:, kc, half * 512:(half + 1) * 512], ["UT0", "Wple"], ["pV"],
                                 start=(kc == 0), stop=(kc == 7))
                    k.act(gt, pV[:].rearrange("p a b -> p (a b)"), AF.Sigmoid, ["pV"], ["Uf0"])
                    k.dma(pf[:], p_own[r0:r0 + 128, :], [], ["pf"])
                    k.cp("vector", pb[:], pf[:], ["pf"], ["pb"])
                    for j in range(2):
                        k.tr(pU[:, j, :], pb[:, j * 128:(j + 1) * 128], identb[:], ["pb", "identb"], ["pU"])
                    k.cp("scalar", pTt[:], pU[:, 0:2, :], ["pU"], ["pTt"])
                    for half in range(2):
                        for j in range(2):
                            k.mm(pGt[:, half, :], pTt[:, j, :], Wple[:, 8 + j, half * 512:(half + 1) * 512],
                                 ["pTt", "Wple"], ["pGt0", "pGt1"], start=(j == 0), stop=(j == 1))
                    k.tt("vector", gt, pGt[:, 0:2, :].rearrange("p a b -> p (a b)"), gt, ALU.mult, ["pGt0", "pGt1", "Uf0"], ["Uf0"])
                    k.tt("vector", x3, gt, acc[:, blk, :], ALU.add, ["Uf0", "acc"], ["Uf0"])
                    o_ = Vf[0][:, blk % 2, :]
                    rmsnorm_bf(nss, nrs, Vf[1][:, 0, :], "Vf1", x3, "Uf0", gfin, "Uf1", o_, "Vf0")
                    k.dma(out[r0:r0 + 128, :], o_, ["Vf0"], ["out"])
                S.emit()
            S.final_wait("sync", ["out"])
            S.emit()
        PW.close()
    return nc


def host_consts(T_ALL):
    NBLK = T_ALL // 128
    own0 = T_ALL - T_OWN
    bf = ml_dtypes.bfloat16
    c = {}
    c["c_identb"] = np.eye(128, dtype=np.float32).astype(bf)
    c["c_identf"] = np.eye(128, dtype=np.float32)
    s = np.arange(128)
    c["c_tri"] = (s[:, None] <= s[None, :]).astype(np.float32)
    r = np.arange(512)
    qaug = np.zeros((3, 4, T_OWN), np.float32)
    for h in range(4):
        for g in range(4):
            qaug[0, h, g * 512:(g + 1) * 512] = -SLOPES[h] * 2.0 * (r // 2)
            qaug[1, h, g * 512:(g + 1) * 512] = -SLOPES[h] * (r % 2)
    qaug[2] = NEG
    c["c_qaug"] = qaug.astype(bf)
    bias = np.zeros((128, 4, 4, NBLK), np.float32)
    p = np.arange(128)
    for h in range(4):
        for g in range(4):
            qstart = own0 + 512 * g
            for kb in range(NBLK):
                bias[:, h, g, kb] = SLOPES[h] * (128 * kb + p - qstart)
    c["c_bias"] = bias
    dm = np.zeros((128, 4, 512), np.float32)
    for j in range(4):
        kp = 128 * j + p
        dm[:, j, :] = np.where(kp[:, None] <= r[None, :], 0.0, NEG)
    c["c_dmask"] = dm
    return c


def core_inputs(inputs, core, T_ALL, n_prev_real):
    x = np.asarray(inputs["x"], np.float32)[0]
    own0 = n_prev_real
    xseq = np.zeros((T_ALL, D), np.float32)
    xseq[T_ALL - T_OWN - n_prev_real:] = x[:own0 + T_OWN]
    npad = T_ALL - T_OWN - n_prev_real
    padrow = np.zeros((1, T_ALL), np.float32)
    padrow[0, :npad] = 1.0
    valid = np.ones((T_ALL,), np.float32)
    valid[:npad] = 0.0
    sq = lambda a: np.ascontiguousarray(np.asarray(a, np.float32)[0])
    m = {
        "xseq": xseq,
        "p_own": np.ascontiguousarray(np.asarray(inputs["p"], np.float32)[0, 0, own0:own0 + T_OWN]),
        "padrow": padrow.astype(ml_dtypes.bfloat16),
        "valid": np.ascontiguousarray(valid.reshape(T_ALL // 128, 128).T),
        "w_in": sq(inputs["w_in"]), "norm_mix_g": sq(inputs["norm_mix_g"]),
        "conv_w": np.ascontiguousarray(sq(inputs["conv_w"]).T.reshape(8, 128, 4).transpose(1, 0, 2)),
        "conv_b": np.ascontiguousarray(sq(inputs["conv_b"]).reshape(8, 128).T),
        "b_gates": np.concatenate([sq(inputs["b_igate"]), sq(inputs["b_fgate"])]),
        "lam4": np.stack([sq(inputs["lambda_q1"]), sq(inputs["lambda_k1"]), sq(inputs["lambda_q2"]), sq(inputs["lambda_k2"])]),
        "da_subln_g": sq(inputs["da_subln_g"]), "ml_norm_g": sq(inputs["ml_norm_g"]).reshape(512),
        "w_out": sq(inputs["w_out"]), "norm_ffn_g": sq(inputs["norm_ffn_g"]),
        "w_query": sq(inputs["w_query"]), "sub_keys": sq(inputs["sub_keys"]).reshape(16, 128, 128),
        "expert_u": sq(inputs["expert_u"]), "expert_v": sq(inputs["expert_v"]),
        "norm_ple_g": sq(inputs["norm_ple_g"]), "w_ple_gate": sq(inputs["w_ple_gate"]),
        "w_ple_proj": sq(inputs["w_ple_proj"]), "final_norm_g": np.asarray(inputs["final_norm_g"], np.float32),
    }
    return m


_NC_CACHE = {}


def kernel(**inputs):
    T_ALL = 16384
    n = 8
    if T_ALL not in _NC_CACHE:
        _NC_CACHE[T_ALL] = build(T_ALL)
    nc = _NC_CACHE[T_ALL]
    consts = host_consts(T_ALL)
    in_maps = []
    for c in range(n):
        m = core_inputs(inputs, c, T_ALL, c * T_OWN)
        m.update(consts)
        in_maps.append(m)
    res = run_bass_kernel_spmd(nc, in_maps, core_ids=list(range(n)))
    out = np.concatenate([np.asarray(r["out"], np.float32) for r in res.results], axis=0)
    return out.reshape(1, n * T_OWN, D)
```

```python
import math
from contextlib import ExitStack
import numpy as np
import ml_dtypes
import concourse.bass as bass
import concourse.mybir as mybir
from concourse.bass_utils import run_bass_kernel_spmd

F32 = mybir.dt.float32
BF16 = mybir.dt.bfloat16
AF = mybir.ActivationFunctionType
ALU = mybir.AluOpType

ENGS = ["tensor", "vector", "scalar", "gpsimd", "sync"]
SAME_ENGINE_SYNC = {"vector": True, "scalar": True, "gpsimd": True, "tensor": False, "sync": False}

D = 1024
T_OWN = 2048
NQB = 16
NEG = -30000.0


class Sched:
    def __init__(self, nc):
        self.nc = nc
        self.lists = {e: [] for e in ENGS}
        self.esem = {e: nc.alloc_semaphore("es_" + e) for e in ENGS if e != "sync"}
        self.cnt = {e: 0 for e in ENGS}
        self.seen = {e: {} for e in ENGS}
        self.writer = {}
        self.readers = {}
        self.dsem = {}
        self.n_dma_sems = 0
        self.nops = 0

    def _wait(self, eng, dep):
        sem, val, src = dep
        if src == eng and not SAME_ENGINE_SYNC.get(eng, False):
            return
        k = id(sem)
        if self.seen[eng].get(k, 0) >= val:
            return
        self.seen[eng][k] = val
        self.lists[eng].append(("wait", sem, val))

    def _deps(self, eng, reads, writes):
        for r in reads:
            for d in self.writer.get(r, {}).values():
                self._wait(eng, d)
            if len(r) > 1 and r[0] == "p" and r[1].isupper():
                for d in self.readers.get(r, ()):
                    if d[2] != eng:
                        self._wait(eng, d)
        for w in writes:
            for d in self.writer.get(w, {}).values():
                self._wait(eng, d)
            for d in self.readers.get(w, ()):
                self._wait(eng, d)

    def _commit(self, dep, reads, writes, merge=False):
        for w in writes:
            if merge:
                self.writer.setdefault(w, {})[id(dep[0])] = dep
            else:
                self.writer[w] = {id(dep[0]): dep}
            self.readers[w] = []
        for r in reads:
            lst = self.readers.setdefault(r, [])
            lst[:] = [d for d in lst if d[0] is not dep[0]]
            lst.append(dep)

    def op(self, eng, fn, reads=(), writes=()):
        self._deps(eng, reads, writes)
        self.cnt[eng] += 1
        self.nops += 1
        dep = (self.esem[eng], self.cnt[eng], eng)
        self.lists[eng].append(("op", fn, self.esem[eng], 1))
        self._commit(dep, reads, writes)

    DRAM_KEYS = ("KTscr", "Vscr", "Yscr", "X1scr", "Wscr", "out")

    def dma(self, fn, reads=(), writes=(), key=None, eng="sync"):
        self._deps(eng, reads, writes)
        if key is None:
            key = reads[0] if (writes and writes[0] in self.DRAM_KEYS and reads) else (writes[0] if writes else reads[0])
        if key not in self.dsem:
            self.dsem[key] = [self.nc.alloc_semaphore("ds%d" % self.n_dma_sems), 0]
            self.n_dma_sems += 1
        ds = self.dsem[key]
        ds[1] += 16
        self.nops += 1
        dep = (ds[0], ds[1], None)
        self.lists[eng].append(("op", fn, ds[0], 16))
        self._commit(dep, reads, writes, merge=True)

    def drain(self):
        for e in ENGS:
            for e2 in ENGS:
                if e2 != "sync" and self.cnt[e2] > 0 and (e2 != e):
                    self._wait(e, (self.esem[e2], self.cnt[e2], e2))
            for ds in self.dsem.values():
                self._wait(e, (ds[0], ds[1], None))

    def final_wait(self, eng, keys):
        for k in keys:
            for d in self.writer.get(k, {}).values():
                self._wait(eng, d)

    def emit(self):
        with self.nc.Block() as block:
            for e in ENGS:
                lst = self.lists[e]
                if not lst:
                    continue

                def body(engh, lst=lst):
                    for it in lst:
                        if it[0] == "wait":
                            engh.wait_ge(it[1], it[2])
                        else:
                            it[1](engh).then_inc(it[2], it[3])

                getattr(block, e)(body)
        self.lists = {e: [] for e in ENGS}


class K:
    def __init__(self, nc, S):
        self.nc, self.S = nc, S

    def mm(self, out, lhsT, rhs, r, w, start=True, stop=True):
        self.S.op("tensor", lambda e: e.matmul(out, lhsT=lhsT, rhs=rhs, start=start, stop=stop,
                                               skip_group_check=True), r, w)

    def tr(self, out, in_, ident, r, w):
        self.S.op("tensor", lambda e: e.transpose(out=out, in_=in_, identity=ident), r, w)

    def act(self, out, in_, func, r, w, **kw):
        self.S.op("scalar", lambda e: e.activation(out=out, in_=in_, func=func, **kw), r, w)

    def ts(self, eng, out, in0, s1, s2, op0, op1, r, w):
        if op1 is None:
            self.S.op(eng, lambda e: e.tensor_scalar(out=out, in0=in0, scalar1=s1, scalar2=None, op0=op0), r, w)
        else:
            self.S.op(eng, lambda e: e.tensor_scalar(out=out, in0=in0, scalar1=s1, scalar2=s2, op0=op0, op1=op1), r, w)

    def tt(self, eng, out, in0, in1, op, r, w):
        self.S.op(eng, lambda e: e.tensor_tensor(out=out, in0=in0, in1=in1, op=op), r, w)

    def stt(self, out, in0, scalar, in1, op0, op1, r, w):
        self.S.op("vector", lambda e: e.scalar_tensor_tensor(out=out, in0=in0, scalar=scalar, in1=in1,
                                                             op0=op0, op1=op1), r, w)

    def cp(self, eng, out, in_, r, w):
        if eng == "scalar":
            self.S.op(eng, lambda e: e.copy(out=out, in_=in_), r, w)
        else:
            self.S.op(eng, lambda e: e.tensor_copy(out=out, in_=in_), r, w)

    def dma(self, out, in_, r, w, key=None, eng="sync"):
        self.S.dma(lambda e: e.dma_start(out=out, in_=in_), r, w, key=key, eng=eng)

    def memset(self, eng, out, val, w):
        self.S.op(eng, lambda e: e.memset(out, val), (), w)


C_QDA, C_KDA, C_VDA, C_MLQ, C_MLK, C_MLV, C_MLO, C_G = 0, 512, 1024, 1536, 2048, 2560, 3072, 3584
SLOPES = [2.0 ** (-2.0 * (i + 1)) for i in range(4)]
LAMBDA_INIT = 0.8 - 0.6 * math.exp(0.0)


def build(T_ALL, stage=99):
    NBLK = T_ALL // 128
    NT = T_ALL // 512
    OWN_BLK0 = NBLK - NQB
    OWN_T0 = NT - 4
    nc = bass.Bass("TRN2", target_bir_lowering=False)

    def din(name, shape, dt=F32):
        return nc.dram_tensor(name, list(shape), dt, kind="ExternalInput").ap()

    xseq = din("xseq", [T_ALL, D])
    p_own = din("p_own", [T_OWN, 256])
    padrow = din("padrow", [1, T_ALL], BF16)
    valid = din("valid", [128, NBLK])
    w_in = din("w_in", [D, 3592])
    norm_mix_g = din("norm_mix_g", [D])
    conv_w = din("conv_w", [128, 8, 4])
    conv_b = din("conv_b", [128, 8])
    b_gates = din("b_gates", [8])
    lam4 = din("lam4", [4, 64])
    da_subln_g = din("da_subln_g", [128])
    ml_norm_g = din("ml_norm_g", [512])
    w_out = din("w_out", [D, D])
    norm_ffn_g = din("norm_ffn_g", [D])
    w_query = din("w_query", [D, 2048])
    sub_keys = din("sub_keys", [16, 128, 128])
    expert_u = din("expert_u", [16384, D])
    expert_v = din("expert_v", [16384, D])
    norm_ple_g = din("norm_ple_g", [D])
    w_ple_gate = din("w_ple_gate", [D, D])
    w_ple_proj = din("w_ple_proj", [256, D])
    final_norm_g = din("final_norm_g", [D])
    c_identb = din("c_identb", [128, 128], BF16)
    c_identf = din("c_identf", [128, 128])
    c_tri = din("c_tri", [128, 128])
    c_qaug = din("c_qaug", [3, 4, T_OWN], BF16)
    c_bias = din("c_bias", [128, 4, 4, NBLK])
    c_dmask = din("c_dmask", [128, 4, 512])
    out = nc.dram_tensor("out", [T_OWN, D], F32, kind="ExternalOutput").ap()

    KTscr = nc.dram_tensor("KTscr", [4, 128, T_ALL], BF16).ap()
    Vscr = nc.dram_tensor("Vscr", [4, 128, NBLK, 128], BF16).ap()
    Yscr = nc.dram_tensor("Yscr", [T_OWN, D], BF16).ap()
    X1scr = nc.dram_tensor("X1scr", [T_OWN, D], F32).ap()

    S = Sched(nc)
    k = K(nc, S)

    with ExitStack() as G:
        def SB(ctx, name, shape, dt=F32):
            return ctx.enter_context(nc.sbuf_tensor(name, list(shape), dt))

        def PS(ctx, name, shape, dt=F32):
            return ctx.enter_context(nc.psum_tensor(name, list(shape), dt))

        identb = SB(G, "identb", [128, 128], BF16)
        identf = SB(G, "identf", [128, 128])
        tri = SB(G, "tri", [128, 128])
        ntri = SB(G, "ntri", [128, 128])
        ones = SB(G, "ones", [128, 128])
        P12 = ExitStack()
        QT0 = SB(P12, "QT0", [67, 4, T_OWN], BF16)
        QT1 = SB(P12, "QT1", [67, 4, T_OWN], BF16)
        lamc = SB(P12, "lamc", [128, 1])
        k.dma(identb[:], c_identb, [], ["identb"])
        k.dma(identf[:], c_identf, [], ["identf"])
        k.dma(tri[:], c_tri, [], ["tri"])
        k.memset("vector", ones[:], 1.0, ["ones"])
        k.ts("vector", ntri[:], tri[:], -1.0, None, ALU.add, None, ["tri"], ["ntri"])
        k.dma(QT0[64:67, :, :], c_qaug, [], ["QT0"])
        k.dma(QT1[64:67, :, :], c_qaug, [], ["QT1"])

        with ExitStack() as P1:
            Wb = SB(P1, "Wb", [128, 8, 3592], BF16)
            gmix = SB(P1, "gmix", [128, D])
            cw = SB(P1, "cw", [128, 8, 4])
            cb = SB(P1, "cb", [128, 8])
            bg = SB(P1, "bg", [128, 8])
            vld = SB(P1, "vld", [128, NBLK])
            gml = SB(P1, "gml", [128, 512])
            gsub = SB(P1, "gsub", [128, 128])
            lamt = SB(P1, "lamt", [128, 4, 64])
            lamp = SB(P1, "lamp", [128, 2, 64])
            lams = SB(P1, "lams", [128, 2])
            State = SB(P1, "State", [128, 4, 129])
            k.dma(gmix[:], norm_mix_g.partition_broadcast(128), [], ["gmix"])
            k.dma(cw[:], conv_w, [], ["cw"])
            k.dma(cb[:], conv_b, [], ["cb"])
            k.dma(bg[:], b_gates.partition_broadcast(128), [], ["bg"])
            k.dma(vld[:], valid, [], ["vld"])
            k.dma(gml[:], ml_norm_g.partition_broadcast(128), [], ["gml"])
            k.dma(gsub[:], da_subln_g.partition_broadcast(128), [], ["gsub"])
            k.dma(lamt[:], lam4.partition_broadcast(128), [], ["lamt"])
            k.memset("vector", State[:], 0.0, ["State"])
            k.tt("vector", lamp[:, 0, :], lamt[:, 0, :], lamt[:, 1, :], ALU.mult, ["lamt"], ["lamp"])
            k.tt("vector", lamp[:, 1, :], lamt[:, 2, :], lamt[:, 3, :], ALU.mult, ["lamt"], ["lamp"])
            S.op("vector", lambda e: e.reduce_sum(out=lams[:], in_=lamp[:], axis=mybir.AxisListType.X), ["lamp"], ["lams"])
            k.act(lams[:], lams[:], AF.Exp, ["lams"], ["lams"])
            k.tt("vector", lamc[:], lams[:, 1:2], lams[:, 0:1], ALU.subtract, ["lams"], ["lamc"])
            k.ts("vector", lamc[:], lamc[:], -LAMBDA_INIT, None, ALU.add, None, ["lamc"], ["lamc"])

            with ExitStack() as W0:
                wst = [SB(W0, "wst%d" % i, [128, 8, 512]) for i in range(2)]
                for ci, c0 in enumerate(range(0, 3592, 512)):
                    c1 = min(c0 + 512, 3592)
                    st = wst[ci % 2]
                    key = "wst%d" % (ci % 2)
                    k.dma(st[:, :, 0:c1 - c0], w_in[:, c0:c1].rearrange("(k p) n -> p k n", p=128), [], [key])
                    k.cp("gpsimd" if ci % 2 else "vector", Wb[:, :, c0:c1], st[:, :, 0:c1 - c0], [key], ["Wb"])
                S.drain()
                S.emit()

            xs = [SB(P1, "xs%d" % i, [128, D]) for i in range(4)]
            sq = SB(P1, "sq", [128, D])
            ss = SB(P1, "ss", [128, 4])
            rs = SB(P1, "rs", [128, 4])
            xb = [SB(P1, "xb%d" % i, [128, D], BF16) for i in range(4)]
            hT = [SB(P1, "hT%d" % i, [128, 8, 515], BF16) for i in range(2)]
            ktst = [SB(P1, "ktst%d" % i, [128, 512], BF16) for i in range(2)]
            vst = [SB(P1, "vst%d" % i, [128, 512], BF16) for i in range(2)]
            kpre = SB(P1, "kpre", [128, 515])
            cacc = SB(P1, "cacc", [128, 512])
            kT = SB(P1, "kT", [128, 4, 512], BF16)
            qT = SB(P1, "qT", [128, 4, 512], BF16)
            kml = [SB(P1, "kml%d" % i, [128, 512], BF16) for i in range(2)]
            vml = [SB(P1, "vml%d" % i, [128, 512], BF16) for i in range(2)]
            rhsW = [SB(P1, "rhsW%d" % i, [128, 4, 129], BF16) for i in range(2)]
            rhsV = SB(P1, "rhsV", [128, 4, 129], BF16)
            gsb = SB(P1, "gsb", [128, 8])
            nlf = SB(P1, "nlf", [128, 4])
            t4 = SB(P1, "t4", [128, 4, 4])
            t4b = SB(P1, "t4b", [128, 4, 4])
            wv = SB(P1, "wv", [128, 4])
            dec = SB(P1, "dec", [128, 4])
            uu = SB(P1, "uu", [128, 4])
            aa = SB(P1, "aa", [128, 4])
            SuT = SB(P1, "SuT", [128, 128], BF16)
            Sbf = SB(P1, "Sbf", [128, 4, 129], BF16)
            dn = SB(P1, "dn", [128, 2])
            hs = SB(P1, "hs", [128, 512])
            og = SB(P1, "og", [128, 512])
            zz = SB(P1, "zz", [128, 512])
            zq = SB(P1, "zq", [128, 128])
            ssm = SB(P1, "ssm", [128, 4])
            rsm = SB(P1, "rsm", [128, 4])
            ymb = [SB(P1, "ymb%d" % i, [128, 512], BF16) for i in range(2)]
            pT = [PS(P1, "pT%d" % i, [128, 8, 128], BF16) for i in range(2)]
            pP = [PS(P1, "pP%d" % i, [128, 512]) for i in range(2)]
            pG = PS(P1, "pG", [128, 512])
            pS = [PS(P1, "pS%d" % i, [128, 2, 256]) for i in range(2)]
            pO = PS(P1, "pO", [128, 512])
            pOb = pO[:].bitcast(BF16)

            k.memset("vector", hT[0][:, :, 0:3], 0.0, ["hT0"])
            k.memset("vector", hT[1][:, :, 0:3], 0.0, ["hT1"])
            k.memset("vector", rhsV[:], 1.0, ["rhsV"])
            ppi = [0]

            def nextP():
                ppi[0] += 1
                i = ppi[0] % 2
                return pP[i], "pP%d" % i

            LNSC = math.log(128.0 ** -0.5)
            gsbA = SB(P1, "gsbA", [128, 4, 8])
            nlfA = SB(P1, "nlfA", [128, 4, 4])
            wvA = SB(P1, "wvA", [128, 4, 4])
            decA = SB(P1, "decA", [128, 4, 4])
            uuA = SB(P1, "uuA", [128, 4, 4])
            aaA = SB(P1, "aaA", [128, 4, 4])

            def normLoad(ti):
                for b in range(4):
                    gb = ti * 4 + b
                    k.dma(xs[b][:], xseq[gb * 128:(gb + 1) * 128, :], [], ["xs%d" % b])

            def normA(ti):
                for b in range(4):
                    k.act(sq[:], xs[b][:], AF.Square, ["xs%d" % b], ["sq", "ss"], accum_out=ss[:, b:b + 1])
                k.act(rs[:], ss[:], AF.Ln, ["ss"], ["rs"], scale=1.0 / D, bias=1e-6)
                k.act(rs[:], rs[:], AF.Exp, ["rs"], ["rs"], scale=-0.5)
                for b in range(4):
                    k.stt(xb[b][:], xs[b][:], rs[:, b:b + 1], gmix[:], ALU.mult, ALU.mult, ["xs%d" % b, "rs", "gmix"], ["xb%d" % b])

            def normB(ti):
                h_ = hT[ti % 2]
                hk = "hT%d" % (ti % 2)
                hp = hT[(ti + 1) % 2]
                hpk = "hT%d" % ((ti + 1) % 2)
                if ti > 0:
                    k.cp("scalar", h_[:, :, 0:3], hp[:, :, 512:515], [hpk], [hk])
                for b in range(4):
                    pt = pT[b % 2]
                    ptk = "pT%d" % (b % 2)
                    for kc in range(8):
                        k.tr(pt[:, kc, :], xb[b][:, kc * 128:(kc + 1) * 128], identb[:], ["xb%d" % b, "identb"], [ptk])
                    k.cp("scalar", h_[:, :, 3 + b * 128:3 + (b + 1) * 128], pt[:], [ptk], [hk])

            def gatesA(ti):
                h_ = hT[ti % 2]
                hk = "hT%d" % (ti % 2)
                for b in range(4):
                    tcs = slice(3 + b * 128, 3 + (b + 1) * 128)
                    g0 = b * 20
                    for kc in range(8):
                        k.mm(pG[:, g0:g0 + 8], h_[:, kc, tcs], Wb[:, kc, C_G:C_G + 8], ["Wb", hk], ["pG"],
                             start=(kc == 0), stop=(kc == 7))
                for b in range(4):
                    g0 = b * 20
                    k.tt("vector", gsbA[:, b, :], pG[:, g0:g0 + 8], bg[:], ALU.add, ["pG", "bg"], ["gsb%d" % b])
                k.act(nlfA[:], gsbA[:, :, 4:8], AF.Exp, ["gsb0", "gsb1", "gsb2", "gsb3"], ["nlf"], scale=-1.0)
                k.act(nlfA[:], nlfA[:], AF.Ln, ["nlf"], ["nlf"], bias=1.0)

            def gatesB(ti):
                own = ti >= OWN_T0
                for b in range(4):
                    g0 = b * 20
                    nl = nlfA[:, b, :]
                    k.mm(pG[:, g0 + 8:g0 + 12], tri[:], nl, ["tri", "nlf"], ["pG"])
                    k.mm(pG[:, g0 + 12:g0 + 16], ntri[:], nl, ["ntri", "nlf"], ["pG"])
                    k.mm(pG[:, g0 + 16:g0 + 20], ones[:], nl, ["ones", "nlf"], ["pG"])
                for b in range(4):
                    gb = ti * 4 + b
                    g0 = b * 20
                    k.tt("vector", t4[:, b, :], pG[:, g0 + 12:g0 + 16], gsbA[:, b, 0:4], ALU.add, ["pG", "gsb%d" % b], ["t4"])
                    k.act(decA[:, b, :], pG[:, g0 + 16:g0 + 20], AF.Exp, ["pG"], ["dec%d" % b], scale=-1.0)
                    if own:
                        k.tt("vector", t4b[:, b, :], pG[:, g0 + 8:g0 + 12], gsbA[:, b, 0:4], ALU.add, ["pG", "gsb%d" % b], ["t4b"])
                        k.act(aaA[:, b, :], pG[:, g0 + 8:g0 + 12], AF.Exp, ["pG"], ["aa%d" % b], scale=-1.0, bias=LNSC)
                k.act(t4[:], t4[:], AF.Exp, ["t4"], ["t4"])
                for b in range(4):
                    gb = ti * 4 + b
                    k.ts("vector", wvA[:, b, :], t4[:, b, :], vld[:, gb:gb + 1], None, ALU.mult, None, ["t4", "vld"], ["wv%d" % b])
                if own:
                    k.act(uuA[:], t4b[:], AF.Exp, ["t4b"], ["uu0", "uu1", "uu2", "uu3"])

            def projK(ti):
                h_ = hT[ti % 2]
                hk = "hT%d" % (ti % 2)
                for h in range(4):
                    pp, ppk = nextP()
                    for kc in range(8):
                        k.mm(pp[:], Wb[:, kc, C_KDA + h * 128:C_KDA + (h + 1) * 128], h_[:, kc, 3:515],
                             ["Wb", hk], [ppk], start=(kc == 0), stop=(kc == 7))
                    st = ktst[h % 2]
                    stk = "ktst%d" % (h % 2)
                    k.cp("vector", st[:], pp[:], [ppk], [stk])
                    k.dma(KTscr[h, :, ti * 512:(ti + 1) * 512], st[:], [stk], ["KTscr"])

            def projQ(ti):
                h_ = hT[ti % 2]
                hk = "hT%d" % (ti % 2)
                qc0 = (ti - OWN_T0) * 512
                for h in range(4):
                    for c in range(2):
                        pp, ppk = nextP()
                        col = C_QDA + h * 128 + c * 64
                        for kc in range(8):
                            k.mm(pp[0:64, :], Wb[:, kc, col:col + 64], h_[:, kc, 3:515], ["Wb", hk], [ppk],
                                 start=(kc == 0), stop=(kc == 7))
                        QT = QT0 if c == 0 else QT1
                        k.act(QT[0:64, h, qc0:qc0 + 512], pp[0:64, :], AF.Copy, [ppk], ["QT%d" % c], scale=0.125)

            def projConv(ti):
                own = ti >= OWN_T0
                h_ = hT[ti % 2]
                hk = "hT%d" % (ti % 2)
                for which in ([1, 0] if own else [1]):
                    cbase = C_MLK if which == 1 else C_MLQ
                    dst = kT if which == 1 else qT
                    dstk = "kT" if which == 1 else "qT"
                    for c in range(4):
                        pp, ppk = nextP()
                        for kc in range(8):
                            k.mm(pp[:], Wb[:, kc, cbase + c * 128:cbase + (c + 1) * 128], h_[:, kc, 3:515],
                                 ["Wb", hk], [ppk], start=(kc == 0), stop=(kc == 7))
                        for kc in range(8):
                            k.mm(pG[:, 96:99], Wb[:, kc, cbase + c * 128:cbase + (c + 1) * 128], h_[:, kc, 0:3],
                                 ["Wb", hk], ["pG"], start=(kc == 0), stop=(kc == 7))
                        k.cp("scalar", kpre[:, 3:515], pp[:], [ppk], ["kpre"])
                        k.cp("scalar", kpre[:, 0:3], pG[:, 96:99], ["pG"], ["kpre"])
                        cc = which * 4 + c
                        k.ts("vector", cacc[:], kpre[:, 0:512], cw[:, cc, 0:1], cb[:, cc:cc + 1], ALU.mult, ALU.add,
                             ["kpre", "cw", "cb"], ["cacc"])
                        for j in range(1, 4):
                            k.stt(cacc[:], kpre[:, j:j + 512], cw[:, cc, j:j + 1], cacc[:], ALU.mult, ALU.add,
                                  ["kpre", "cw", "cacc"], ["cacc"])
                        k.act(dst[:, c, :], cacc[:], AF.Silu, ["cacc"], [dstk])

            def blocks(ti):
                own = ti >= OWN_T0
                h_ = hT[ti % 2]
                hk = "hT%d" % (ti % 2)
                for b in range(4):
                    gb = ti * 4 + b
                    tcs = slice(3 + b * 128, 3 + (b + 1) * 128)
                    wv, wvk = wvA[:, b, :], "wv%d" % b
                    dec, deck = decA[:, b, :], "dec%d" % b
                    uu, uuk = uuA[:, b, :], "uu%d" % b
                    aa, aak = aaA[:, b, :], "aa%d" % b
                    pp, ppk = nextP()
                    for kc in range(8):
                        k.mm(pp[:], h_[:, kc, tcs], Wb[:, kc, C_MLV:C_MLV + 512], ["Wb", hk], [ppk],
                             start=(kc == 0), stop=(kc == 7))
                    vm = vml[gb % 2]
                    vmk = "vml%d" % (gb % 2)
                    k.cp("scalar", vm[:], pp[:], [ppk], [vmk])
                    for h in range(4):
                        k.tr(pOb[:, h * 128:(h + 1) * 128], kT[:, h, b * 128:(b + 1) * 128], identb[:],
                             ["kT", "identb"], ["pO"])
                    km = kml[gb % 2]
                    kmk = "kml%d" % (gb % 2)
                    k.cp("vector", km[:], pOb[:, 0:512], ["pO"], [kmk])
                    rw = rhsW[gb % 2]
                    rwk = "rhsW%d" % (gb % 2)
                    for h in range(4):
                        k.act(rw[:, h, 0:128], vm[:, h * 128:(h + 1) * 128], AF.Copy, [vmk, wvk], [rwk], scale=wv[:, h:h + 1])
                    k.cp("scalar", rw[:, :, 128:129], wv.unsqueeze(2), [wvk], [rwk])
                    pp, ppk = nextP()
                    for kc in range(8):
                        k.mm(pp[:], h_[:, kc, tcs], Wb[:, kc, C_VDA:C_VDA + 512], ["Wb", hk], [ppk],
                             start=(kc == 0), stop=(kc == 7))
                    st = vst[gb % 2]
                    stk = "vst%d" % (gb % 2)
                    k.cp("vector", st[:], pp[:], [ppk], [stk])
                    k.dma(Vscr[:, :, gb, :].rearrange("h p d -> p h d"), st[:].rearrange("p (h d) -> p h d", h=4),
                          [stk], ["Vscr"])
                    if own:
                        qb = gb - OWN_BLK0
                        k.cp("scalar", rhsV[:, :, 0:128], vm[:].rearrange("p (h d) -> p h d", h=4), [vmk], ["rhsV"])
                        k.cp("scalar", Sbf[:], State[:], ["State"], ["Sbf"])
                        pp, ppk = nextP()
                        for kc in range(8):
                            k.mm(pp[:], h_[:, kc, tcs], Wb[:, kc, C_MLO:C_MLO + 512], ["Wb", hk], [ppk],
                                 start=(kc == 0), stop=(kc == 7))
                        k.act(og[:], pp[:], AF.Sigmoid, [ppk], ["og"])
                        for h in range(4):
                            bs = slice(b * 128, (b + 1) * 128)
                            k.mm(pO[:, 0:128], kT[:, h, bs], qT[:, h, bs], ["kT", "qT"], ["pO"])
                            k.stt(SuT[:], pO[:, 0:128], uu[:, h:h + 1], tri[:], ALU.mult, ALU.mult,
                                  ["pO", uuk, "tri"], ["SuT"])
                            k.mm(pO[:, 256:385], SuT[:], rhsV[:, h, :], ["SuT", "rhsV"], ["pO"], start=True, stop=False)
                            k.mm(pO[:, 256:385], qT[:, h, bs], Sbf[:, h, :], ["qT", "Sbf"], ["pO"], start=False, stop=True)
                            k.tt("vector", dn[:, 0:1], pO[:, 384:385], aa[:, h:h + 1], ALU.mult, ["pO", aak], ["dn"])
                            k.ts("vector", dn[:, 1:2], dn[:, 0:1], -1.0, None, ALU.mult, None, ["dn"], ["dn"])
                            k.tt("vector", dn[:, 0:1], dn[:, 0:1], dn[:, 1:2], ALU.max, ["dn"], ["dn"])
                            k.ts("vector", dn[:, 0:1], dn[:, 0:1], 1.0, None, ALU.max, None, ["dn"], ["dn"])
                            S.op("vector", lambda e: e.reciprocal(out=dn[:, 0:1], in_=dn[:, 0:1]), ["dn"], ["dn"])
                            k.tt("vector", dn[:, 1:2], dn[:, 0:1], aa[:, h:h + 1], ALU.mult, ["dn", aak], ["dn"])
                            k.ts("vector", hs[:, h * 128:(h + 1) * 128], pO[:, 256:384], dn[:, 1:2], None, ALU.mult, None,
                                 ["pO", "dn"], ["hs"])
                        k.tt("vector", zz[:], og[:], hs[:], ALU.mult, ["og", "hs"], ["zz"])
                        for h in range(4):
                            k.act(zq[:], zz[:, h * 128:(h + 1) * 128], AF.Square, ["zz"], ["zq", "ssm"],
                                  accum_out=ssm[:, h:h + 1])
                        k.act(rsm[:], ssm[:], AF.Ln, ["ssm"], ["rsm"], scale=1.0 / 128, bias=1e-6)
                        k.act(rsm[:], rsm[:], AF.Exp, ["rsm"], ["rsm"], scale=-0.5)
                        ym = ymb[qb % 2]
                        ymk = "ymb%d" % (qb % 2)
                        for h in range(4):
                            hsl = slice(h * 128, (h + 1) * 128)
                            k.stt(ym[:, hsl], zz[:, hsl], rsm[:, h:h + 1], gml[:, hsl], ALU.mult, ALU.mult,
                                  ["zz", "rsm", "gml"], [ymk])
                        k.dma(Yscr[qb * 128:(qb + 1) * 128, 512:1024], ym[:], [ymk], ["Yscr"])
                    for h in range(4):
                        ps = pS[h // 2]
                        psk = "pS%d" % (h // 2)
                        k.mm(ps[:, h % 2, 0:129], km[:, h * 128:(h + 1) * 128], rw[:, h, :], [kmk, rwk], [psk])
                    for h in range(4):
                        ps = pS[h // 2]
                        psk = "pS%d" % (h // 2)
                        k.stt(State[:, h, :], State[:, h, :], dec[:, h:h + 1], ps[:, h % 2, 0:129], ALU.mult, ALU.add,
                              ["State", deck, psk], ["State"])

            normLoad(0)
            normA(0)
            normB(0)
            for ti in range(NT):
                own = ti >= OWN_T0
                if ti + 1 < NT:
                    normLoad(ti + 1)
                gatesA(ti)
                projConv(ti)
                projK(ti)
                gatesB(ti)
                if ti + 1 < NT:
                    normA(ti + 1)
                if own:
                    projQ(ti)
                if ti + 1 < NT:
                    normB(ti + 1)
                blocks(ti)
            S.drain()
            S.emit()
        with ExitStack() as P2:
            KT0 = SB(P2, "KT0", [67, T_ALL], BF16)
            KT1 = SB(P2, "KT1", [67, T_ALL], BF16)
            Vh = SB(P2, "Vh", [128, NBLK, 129], BF16)
            btab = SB(P2, "btab", [128, 4, 4, NBLK])
            dmask = SB(P2, "dmask", [128, 4, 512])
            gsub2 = SB(P2, "gsub2", [128, 128])
            Pm = [SB(P2, "P%d" % i, [128, 2, 512], BF16) for i in range(3)]
            Sm = SB(P2, "Sm", [128, 2, 512])
            rl = SB(P2, "rl", [128, 2])
            od = SB(P2, "od", [128, 128])
            oq = SB(P2, "oq", [128, 128])
            sso = SB(P2, "sso", [128, 1])
            yda = [SB(P2, "yda%d" % i, [128, 128], BF16) for i in range(2)]
            pSm = [PS(P2, "pSm%d" % i, [128, 2, 512]) for i in range(2)]
            pOa = [PS(P2, "pOa%d" % i, [128, 512]) for i in range(3)]
            k.dma(btab[:], c_bias, [], ["btab"])
            k.dma(dmask[:], c_dmask, [], ["dmask"])
            k.dma(gsub2[:], da_subln_g.partition_broadcast(128), [], ["gsub2"])
            k.memset("vector", KT0[64:66, :], 1.0, ["KT0"])
            k.memset("vector", KT1[64:66, :], 1.0, ["KT1"])
            k.dma(KT0[66:67, :], padrow, [], ["KT0"])
            k.dma(KT1[66:67, :], padrow, [], ["KT1"])
            k.memset("vector", Vh[:, :, 128:129], 1.0, ["Vh"])
            it = 0
            for h in range(4):
                k.dma(KT0[0:64, :], KTscr[h, 0:64, :], ["KTscr"], ["KT0"])
                k.dma(KT1[0:64, :], KTscr[h, 64:128, :], ["KTscr"], ["KT1"])
                k.dma(Vh[:, :, 0:128], Vscr[h], ["Vscr"], ["Vh"])
                for g in range(4):
                    nkb = OWN_BLK0 + 4 * g + 4
                    started = [False, False, False]
                    def qk(kb, i):
                        for m in range(2):
                            KT = KT0 if m == 0 else KT1
                            QT = QT0 if m == 0 else QT1
                            k.mm(pSm[i][:, m, :], KT[0:67, kb * 128:(kb + 1) * 128], QT[0:67, h, g * 512:(g + 1) * 512],
                                 ["KT%d" % m, "QT%d" % m], ["pSm%d" % i])

                    def pv(kb):
                        pi = kb % 3
                        for qb in range(4):
                            for m in range(2):
                                e = qb * 2 + m
                                bank, off = e // 3, (e % 3) * 129
                                st_ = not started[bank]
                                started[bank] = True
                                k.mm(pOa[bank][:, off:off + 129], Pm[pi][:, m, qb * 128:(qb + 1) * 128], Vh[:, kb, :],
                                     ["P%d" % pi, "Vh"], ["pOa%d" % bank], start=st_, stop=(kb == nkb - 1))

                    qk(0, it % 2)
                    for kb in range(nkb):
                        diag = kb >= OWN_BLK0 + 4 * g
                        i = it % 2
                        it += 1
                        pi = kb % 3
                        src, srck = pSm[i][:], "pSm%d" % i
                        if diag:
                            dm_ = dmask[:, kb - (OWN_BLK0 + 4 * g), :]
                            k.tt("vector", Sm[:], src, dm_.unsqueeze(1).broadcast_to([128, 2, 512]), ALU.add,
                                 [srck, "dmask"], ["Sm"])
                            src, srck = Sm[:], "Sm"
                        k.act(Pm[pi][:], src, AF.Exp, [srck, "btab"], ["P%d" % pi], bias=btab[:, h, g, kb:kb + 1])
                        if kb + 1 < nkb:
                            qk(kb + 1, it % 2)
                        if kb >= 1:
                            pv(kb - 1)
                    pv(nkb - 1)
                    for qb in range(4):
                        e0, e1 = qb * 2, qb * 2 + 1
                        b0, o0 = e0 // 3, (e0 % 3) * 129
                        b1, o1 = e1 // 3, (e1 % 3) * 129
                        S.op("vector", lambda e, o=rl[:, 0:1], i_=pOa[b0][:, o0 + 128:o0 + 129]: e.reciprocal(out=o, in_=i_),
                             ["pOa%d" % b0], ["rl"])
                        S.op("vector", lambda e, o=rl[:, 1:2], i_=pOa[b1][:, o1 + 128:o1 + 129]: e.reciprocal(out=o, in_=i_),
                             ["pOa%d" % b1], ["rl"])
                        k.tt("vector", rl[:, 1:2], rl[:, 1:2], lamc[:], ALU.mult, ["rl", "lamc"], ["rl"])
                        k.ts("vector", od[:], pOa[b0][:, o0:o0 + 128], rl[:, 0:1], None, ALU.mult, None,
                             ["pOa%d" % b0, "rl"], ["od"])
                        k.stt(od[:], pOa[b1][:, o1:o1 + 128], rl[:, 1:2], od[:], ALU.mult, ALU.add,
                              ["pOa%d" % b1, "rl", "od"], ["od"])
                        k.act(oq[:], od[:], AF.Square, ["od"], ["oq", "sso"], accum_out=sso[:])
                        k.act(sso[:], sso[:], AF.Ln, ["sso"], ["sso"], scale=1.0 / 128, bias=1e-6)
                        k.act(sso[:], sso[:], AF.Exp, ["sso"], ["sso"], scale=-0.5)
                        k.ts("vector", sso[:], sso[:], 1.0 - LAMBDA_INIT, None, ALU.mult, None, ["sso"], ["sso"])
                        qg = g * 4 + qb
                        yd = yda[qg % 2]
                        ydk = "yda%d" % (qg % 2)
                        k.stt(yd[:], od[:], sso[:], gsub2[:], ALU.mult, ALU.mult, ["od", "sso", "gsub2"], [ydk])
                        k.dma(Yscr[qg * 128:(qg + 1) * 128, h * 128:(h + 1) * 128], yd[:], [ydk], ["Yscr"])
            S.drain()
            S.emit()

        P12.close()
        if stage == 2:
            with ExitStack() as PD:
                yb = SB(PD, "dbg_yb", [128, NQB, D], BF16)
                yf = SB(PD, "dbg_yf", [128, NQB, D])
                k.dma(yb[:], Yscr.rearrange("(q p) d -> p q d", p=128), ["Yscr"], ["dbg_yb"])
                k.cp("vector", yf[:], yb[:], ["dbg_yb"], ["dbg_yf"])
                k.dma(out.rearrange("(q p) d -> p q d", p=128), yf[:], ["dbg_yf"], ["out"])
                S.final_wait("sync", ["out"])
                S.emit()
            return nc

        own_row0 = T_ALL - T_OWN

        def rmsnorm_bf(ctx_ss, ctx_rs, junk, junkk, src, srck, gB, gk, dst, dstk):
            k.act(junk, src, AF.Square, [srck], [junkk, "nss"], accum_out=ctx_ss[:])
            k.act(ctx_rs[:], ctx_ss[:], AF.Ln, ["nss"], ["nrs"], scale=1.0 / D, bias=1e-6)
            k.act(ctx_rs[:], ctx_rs[:], AF.Exp, ["nrs"], ["nrs"], scale=-0.5)
            k.stt(dst, src, ctx_rs[:], gB, ALU.mult, ALU.mult, [srck, "nrs", gk], [dstk])

        with ExitStack() as P3:
            Wo = SB(P3, "Wo", [128, 8, D], BF16)
            yb = [SB(P3, "yb%d" % i, [128, D], BF16) for i in range(2)]
            yT = [SB(P3, "yT%d" % i, [128, 8, 128], BF16) for i in range(2)]
            xr = [SB(P3, "xr%d" % i, [128, D]) for i in range(2)]
            x1 = [SB(P3, "x1_%d" % i, [128, D]) for i in range(2)]
            pT3 = [PS(P3, "pT3_%d" % i, [128, 8, 128], BF16) for i in range(2)]
            pY = [PS(P3, "pY%d" % i, [128, 2, 512]) for i in range(2)]
            for c0 in range(0, D, 512):
                k.dma(Wo[:, :, c0:c0 + 512], w_out[:, c0:c0 + 512].rearrange("(k p) n -> p k n", p=128), [], ["Wo"], eng="gpsimd")
            for qb in range(NQB):
                i = qb % 2
                k.dma(yb[i][:], Yscr[qb * 128:(qb + 1) * 128, :], ["Yscr"], ["yb%d" % i])
                k.dma(xr[i][:], xseq[own_row0 + qb * 128:own_row0 + (qb + 1) * 128, :], [], ["xr%d" % i])
                for kc in range(8):
                    k.tr(pT3[i][:, kc, :], yb[i][:, kc * 128:(kc + 1) * 128], identb[:], ["yb%d" % i, "identb"], ["pT3_%d" % i])
                k.cp("scalar", yT[i][:], pT3[i][:], ["pT3_%d" % i], ["yT%d" % i])
                for half in range(2):
                    for kc in range(8):
                        k.mm(pY[i][:, half, :], yT[i][:, kc, :], Wo[:, kc, half * 512:(half + 1) * 512],
                             ["yT%d" % i, "Wo"], ["pY%d" % i], start=(kc == 0), stop=(kc == 7))
                k.tt("vector", x1[i][:], pY[i][:].rearrange("p a b -> p (a b)"), xr[i][:], ALU.add,
                     ["pY%d" % i, "xr%d" % i], ["x1_%d" % i])
                k.dma(X1scr[qb * 128:(qb + 1) * 128, :], x1[i][:], ["x1_%d" % i], ["X1scr"])
            S.drain()
            S.emit()

        if stage == 3:
            with ExitStack() as PD:
                yf = SB(PD, "dbg_yf", [128, NQB, D])
                k.dma(yf[:], X1scr.rearrange("(q p) d -> p q d", p=128), ["X1scr"], ["dbg_yf"])
                k.dma(out.rearrange("(q p) d -> p q d", p=128), yf[:], ["dbg_yf"], ["out"])
                S.final_wait("sync", ["out"])
                S.emit()
            return nc

        with ExitStack() as P4:
            Wsh = SB(P4, "Wsh", [128, 8, 2048], BF16)
            Wple = SB(P4, "Wple", [128, 10, 1024], BF16)
            gffn = SB(P4, "gffn", [128, D])
            subkT = SB(P4, "subkT", [128, 16, 128], BF16)
            s1t = [SB(P4, "s1t%d" % i, [128, 128]) for i in range(2)]
            wt = SB(P4, "wt", [128, 4, 8])
            E0 = SB(P4, "E0", [128, 4, 8, 128])
            E1 = SB(P4, "E1", [128, 4, 8, 128])
            acc = SB(P4, "acc", [128, 4, D])
            xnT = SB(P4, "xnT", [128, 8, 512], BF16)
            Uf = [SB(P4, "Uf%d" % i, [128, 2, D]) for i in range(2)]
            Vf = [SB(P4, "Vf0", [128, 2, D]), SB(P4, "Vf1", [128, 1, D])]
            Ub = [SB(P4, "Ub%d" % i, [128, 2, D], BF16) for i in range(2)]
            Vb = [SB(P4, "Vb%d" % i, [128, 2, D], BF16) for i in range(3)]
            UT = [SB(P4, "UT%d" % i, [128, 8, 128], BF16) for i in range(2)]
            gA = [[SB(P4, "gA%d_%d" % (i, j), [128, 512], BF16) for j in range(2)] for i in range(2)]
            GA = [[SB(P4, "GA%d_%d" % (i, j), [128, 512], BF16) for j in range(2)] for i in range(2)]
            NW, NT2 = 7, 16
            Wp = [SB(P4, "Wp%d" % i, [128, 2, 128]) for i in range(NW)]
            T2 = [SB(P4, "T2_%d" % i, [128, 2, 128], BF16) for i in range(NT2)]
            nss = SB(P4, "nss", [128, 1])
            nrs = SB(P4, "nrs", [128, 1])
            xnb = SB(P4, "xnb", [128, D], BF16)
            qTs = [SB(P4, "qTs%d" % i, [128, 512], BF16) for i in range(2)]
            s0t = [SB(P4, "s0t%d" % i, [128, 128]) for i in range(2)]
            tmp = [SB(P4, "tmpk%d" % i, [128, 256]) for i in range(2)]
            v16 = [SB(P4, "v16_%d" % i, [128, 2, 16]) for i in range(2)]
            cand = [SB(P4, "cand%d" % i, [128, 256]) for i in range(2)]
            t16 = [SB(P4, "t16_%d" % i, [128, 16]) for i in range(2)]
            sm = [SB(P4, "sm%d" % i, [128, 8]) for i in range(2)]
            skf = SB(P4, "skf", [128, 128])
            skb = SB(P4, "skb", [128, 128], BF16)
            pf = SB(P4, "pf", [128, 256])
            pb = SB(P4, "pb", [128, 256], BF16)
            pTt = SB(P4, "pTt", [128, 2, 128], BF16)
            pQ = PS(P4, "pQ", [128, 512])
            pU = pQ[:].bitcast(BF16).rearrange("p (k e) -> p k e", k=8)
            pA = PS(P4, "pA", [128, 512])
            pGt = PS(P4, "pGt", [128, 4, 512])
            pV = PS(P4, "pV", [128, 2, 512])
            k.dma(gffn[:], norm_ffn_g.partition_broadcast(128), [], ["gffn"])
            for hc in range(16):
                k.dma(skf[:], sub_keys[hc], [], ["skf"])
                k.cp("vector", skb[:], skf[:], ["skf"], ["skb"])
                k.tr(pU[:, 0, :], skb[:], identb[:], ["skb", "identb"], ["pU"])
                k.cp("scalar", subkT[:, hc, :], pU[:, 0, :], ["pU"], ["subkT"])
            S.emit()
            for c0 in range(0, 2048, 512):
                k.dma(Wsh[:, :, c0:c0 + 512], w_query[:, c0:c0 + 512].rearrange("(k p) n -> p k n", p=128), [], ["Wsh"],
                      eng="gpsimd")
            for c0 in range(0, 1024, 512):
                k.dma(Wple[:, 0:8, c0:c0 + 512], w_ple_gate[:, c0:c0 + 512].rearrange("(k p) n -> p k n", p=128), [], ["Wple"],
                      eng="gpsimd")
                k.dma(Wple[:, 8:10, c0:c0 + 512], w_ple_proj[:, c0:c0 + 512].rearrange("(k p) n -> p k n", p=128), [], ["Wple"],
                      eng="gpsimd")
            for qt in range(4):
                for blk in range(4):
                    r0 = (qt * 4 + blk) * 128
                    k.dma(acc[:, blk, :], X1scr[r0:r0 + 128, :], ["X1scr"], ["acc"])
                    rmsnorm_bf(nss, nrs, Vf[1][:, 0, :], "Vf1", acc[:, blk, :], "acc", gffn[:], "gffn", xnb[:], "xnb")
                    for kc in range(8):
                        k.tr(pU[:, kc, :], xnb[:, kc * 128:(kc + 1) * 128], identb[:], ["xnb", "identb"], ["pU"])
                    k.cp("scalar", xnT[:, :, blk * 128:(blk + 1) * 128], pU, ["pU"], ["xnT"])
                for h in range(8):
                    for c in range(2):
                        hc = h * 2 + c
                        for kc in range(8):
                            k.mm(pQ[:], Wsh[:, kc, hc * 128:(hc + 1) * 128], xnT[:, kc, :], ["Wsh", "xnT"], ["pU"],
                                 start=(kc == 0), stop=(kc == 7))
                        k.cp("scalar", qTs[c][:], pQ[:], ["pU"], ["qTs%d" % c])
                    def chain_ops(blk, h, p):
                        bs = slice(blk * 128, (blk + 1) * 128)
                        P = str(p)
                        s0_, s1_, v_, c_, t_, tm_, sm_ = s0t[p], s1t[p], v16[p], cand[p], t16[p], tmp[p], sm[p]
                        o = []
                        o.append(lambda: k.mm(pA[:, p * 256:p * 256 + 128], qTs[0][:, bs], subkT[:, h * 2, :], ["qTs0", "subkT"], ["pA"]))
                        o.append(lambda: k.mm(pA[:, p * 256 + 128:p * 256 + 256], qTs[1][:, bs], subkT[:, h * 2 + 1, :], ["qTs1", "subkT"], ["pA"]))
                        o.append(lambda: k.cp("vector", s0_[:], pA[:, p * 256:p * 256 + 128], ["pA"], ["s0t" + P]))
                        o.append(lambda: k.cp("vector", s1_[:], pA[:, p * 256 + 128:p * 256 + 256], ["pA"], ["s1t" + P]))
                        for c, src, sk in ((0, s0_, "s0t" + P), (1, s1_, "s1t" + P)):
                            o.append(lambda c=c, src=src, sk=sk: S.op("vector", lambda e: e.max(out=v_[:, c, 0:8], in_=src[:]), [sk], ["v16" + P]))
                            o.append(lambda c=c, src=src, sk=sk: S.op("vector", lambda e: e.match_replace(
                                out=tm_[:, 0:128], in_to_replace=v_[:, c, 0:8], in_values=src[:], imm_value=-1e30), ["v16" + P, sk], ["tmpk" + P]))
                            o.append(lambda c=c: S.op("vector", lambda e: e.max(out=v_[:, c, 8:16], in_=tm_[:, 0:128]), ["tmpk" + P], ["v16" + P]))
                        o.append(lambda: k.tt("vector", c_[:].rearrange("p (a b) -> p a b", a=16),
                                              v_[:, 0, :].unsqueeze(2).broadcast_to([128, 16, 16]),
                                              v_[:, 1, :].unsqueeze(1).broadcast_to([128, 16, 16]), ALU.add, ["v16" + P], ["cand" + P]))
                        o.append(lambda: S.op("vector", lambda e: e.max(out=t_[:, 0:8], in_=c_[:]), ["cand" + P], ["t16" + P]))
                        o.append(lambda: S.op("vector", lambda e: e.match_replace(out=tm_[:], in_to_replace=t_[:, 0:8], in_values=c_[:],
                                                                                  imm_value=-1e30), ["t16" + P, "cand" + P], ["tmpk" + P]))
                        o.append(lambda: S.op("vector", lambda e: e.max(out=t_[:, 8:16], in_=tm_[:]), ["tmpk" + P], ["t16" + P]))
                        o.append(lambda: k.ts("vector", sm_[:, 0:1], t_[:, 0:1], -1.0, None, ALU.mult, None, ["t16" + P], ["sm" + P]))
                        o.append(lambda: k.ts("vector", sm_[:, 2:4], v_[:, :, 0], -1.0, None, ALU.mult, None, ["v16" + P], ["sm" + P]))
                        o.append(lambda: k.act(c_[:, 0:16], t_[:], AF.Exp, ["t16" + P, "sm" + P], ["cand" + P, "smz" + P],
                                               bias=sm_[:, 0:1], accum_out=sm_[:, 4:5]))
                        o.append(lambda: S.op("vector", lambda e: e.reciprocal(out=sm_[:, 5:6], in_=sm_[:, 4:5]), ["smz" + P], ["smr" + P]))
                        o.append(lambda: k.ts("vector", wt[:, blk, h:h + 1], c_[:, 15:16], sm_[:, 5:6], 0.9997, ALU.mult, ALU.mult,
                                              ["cand" + P, "smr" + P], ["wt"]))
                        o.append(lambda: k.act(E0[:, blk, h, :], s0_[:], AF.Exp, ["s0t" + P, "sm" + P], ["E0"], bias=sm_[:, 2:3]))
                        o.append(lambda: k.ts("vector", E0[:, blk, h, :], E0[:, blk, h, :], sm_[:, 5:6], None, ALU.mult, None,
                                              ["E0", "smr" + P], ["E0"]))
                        o.append(lambda: k.act(E1[:, blk, h, :], s1_[:], AF.Exp, ["s1t" + P, "sm" + P], ["E1"], bias=sm_[:, 3:4]))
                        return o

                    for bp in range(2):
                        A_ = chain_ops(2 * bp, h, 0)
                        B_ = chain_ops(2 * bp + 1, h, 1)
                        for a_, b_ in zip(A_, B_):
                            a_()
                            b_()
                def eload(sc, whichs):
                    rows = slice(sc * 256, (sc + 1) * 256)
                    if "u" in whichs:
                        k.dma(Ub[sc % 2][:], expert_u[rows, :].rearrange("(s p) d -> p s d", p=128), [], ["Ub%d" % (sc % 2)],
                              eng="gpsimd")
                    if "v" in whichs:
                        k.dma(Vb[sc % 3][:], expert_v[rows, :].rearrange("(s p) d -> p s d", p=128), [], ["Vb%d" % (sc % 3)],
                              eng="gpsimd")

                def pro_steps(sc):
                    sl = sc % 2
                    steps = []
                    for sub in range(2):
                        def s_tr(sub=sub):
                            for kc in range(8):
                                k.tr(pU[:, kc, :], Ub[sl][:, sub, kc * 128:(kc + 1) * 128], identb[:], ["Ub%d" % sl, "identb"], ["pU"])
                        def s_cp(sub=sub):
                            k.cp("scalar", UT[sub][:], pU, ["pU"], ["UT%d" % sub])
                        def s_mmA(sub=sub):
                            for kc in range(0, 4):
                                k.mm(pA[:], UT[sub][:, kc, :], xnT[:, kc, :], ["UT%d" % sub, "xnT"], ["pA"],
                                     start=(kc == 0), stop=(kc == 7))
                        def s_mmB(sub=sub):
                            for kc in range(4, 8):
                                k.mm(pA[:], UT[sub][:, kc, :], xnT[:, kc, :], ["UT%d" % sub, "xnT"], ["pA"],
                                     start=(kc == 0), stop=(kc == 7))
                        def s_ge(sub=sub):
                            k.act(gA[sl][sub][:], pA[:], AF.Gelu, ["pA"], ["gA%d_%d" % (sl, sub)])
                        steps += [s_tr, s_cp, s_mmA, s_mmB, s_ge]
                    return steps

                eload(0, "uv")
                eload(1, "u")
                for st in pro_steps(0):
                    st()
                def epi_steps(sc):
                    sl3 = sc % 3
                    gp = sc % 2
                    steps = []
                    for blk in range(4):
                        def e_mm(half, blk=blk):
                            for sub in range(2):
                                k.mm(pV[:, half, :], GA[gp][sub][:, blk * 128:(blk + 1) * 128],
                                     Vb[sl3][:, sub, half * 512:(half + 1) * 512],
                                     ["GA%d_%d" % (gp, sub), "Vb%d" % sl3], ["pV"], start=(sub == 0), stop=(sub == 1))
                        def e_add(blk=blk):
                            k.tt("vector", acc[:, blk, :], pV[:].rearrange("p a b -> p (a b)"), acc[:, blk, :], ALU.add,
                                 ["pV", "acc"], ["acc"])
                        steps += [lambda e_mm=e_mm: e_mm(0), lambda e_mm=e_mm: e_mm(1), e_add]
                    return steps

                def ga_mult(sc, sub):
                    gp = sc % 2
                    k.tt("vector", GA[gp][sub][:], pGt[:, gp * 2 + sub, :], gA[gp][sub][:], ALU.mult,
                         ["pGt%d" % (gp * 2 + sub), "gA%d_%d" % (gp, sub)], ["GA%d_%d" % (gp, sub)])

                ui = 0
                wi = 0
                for sc in range(64):
                    if sc + 2 < 64:
                        eload(sc + 2, "u")
                    if sc + 1 < 64:
                        eload(sc + 1, "v")
                    pst = pro_steps(sc + 1) if sc + 1 < 64 else [lambda: None] * 10
                    est = epi_steps(sc - 1) if sc > 0 else [lambda: None] * 12
                    sched_ = {0: [pst[0]], 8: [pst[1]], 10: [est[0]], 12: [est[1]], 14: [pst[2]], 16: [est[2], pst[5]],
                              18: [pst[3]], 20: [est[3]], 22: [est[4]], 24: [pst[6]], 26: [est[5]], 28: [pst[4]],
                              30: [est[6]], 32: [est[7]], 34: [pst[7]], 36: [est[8]], 38: [pst[8]], 40: [est[9]],
                              42: [est[10]], 46: [est[11]], 50: [pst[9]]}
                    if sc > 0:
                        sched_[2] = [lambda sc=sc: ga_mult(sc - 1, 0)]
                        sched_[4] = [lambda sc=sc: ga_mult(sc - 1, 1)]
                    gp = sc % 2
                    un = 0
                    for blk in range(4):
                        for h in range(8):
                            for u_ in (un, un + 1):
                                for f_ in sched_.get(u_, ()):
                                    f_()
                            un += 2
                            j, j2 = ui % NW, ui % NT2
                            ui += 1
                            for sub in range(2):
                                n1 = sc * 2 + sub
                                wi += 1
                                if wi % 4 == 0:
                                    k.ts("vector", Wp[j][:, sub, :], E1[:, blk, h, :], E0[:, blk, h, n1:n1 + 1], None, ALU.mult, None,
                                         ["E1", "E0"], ["Wp%d" % j])
                                else:
                                    k.act(Wp[j][:, sub, :], E1[:, blk, h, :], AF.Copy, ["E1", "E0"], ["Wp%d" % j],
                                          scale=E0[:, blk, h, n1:n1 + 1])
                            k.stt(T2[j2][:], Wp[j][:], wt[:, blk, h:h + 1], Wp[j][:], ALU.is_ge, ALU.mult,
                                  ["Wp%d" % j, "wt"], ["T2_%d" % j2])
                            for sub in range(2):
                                k.mm(pGt[:, gp * 2 + sub, blk * 128:(blk + 1) * 128], T2[j2][:, sub, :], identb[:],
                                     ["T2_%d" % j2, "identb"], ["pGt%d" % (gp * 2 + sub)], start=(h == 0), stop=(h == 7))
                ga_mult(63, 0)
                ga_mult(63, 1)
                for f_ in epi_steps(63):
                    f_()
                gt, x3 = Uf[0][:, 0, :], Uf[0][:, 1, :]
                gple, gfin = Uf[1][:, 0, :], Uf[1][:, 1, :]
                k.dma(gple, norm_ple_g.partition_broadcast(128), [], ["Uf1"])
                k.dma(gfin, final_norm_g.partition_broadcast(128), [], ["Uf1"])
                for blk in range(4):
                    r0 = (qt * 4 + blk) * 128
                    rmsnorm_bf(nss, nrs, Vf[1][:, 0, :], "Vf1", acc[:, blk, :], "acc", gple, "Uf1", xnb[:], "xnb")
                    for kc in range(8):
                        k.tr(pU[:, kc, :], xnb[:, kc * 128:(kc + 1) * 128], identb[:], ["xnb", "identb"], ["pU"])
                    k.cp("scalar", UT[0][:], pU, ["pU"], ["UT0"])
                    for half in range(2):
                        for kc in range(8):
                            k.mm(pV[:, half, :], UT[0][:, kc, :], Wple[:, kc, half * 512:(half + 1) * 512], ["UT0", "Wple"], ["pV"],
                                 start=(kc == 0), stop=(kc == 7))
                    k.act(gt, pV[:].rearrange("p a b -> p (a b)"), AF.Sigmoid, ["pV"], ["Uf0"])
                    k.dma(pf[:], p_own[r0:r0 + 128, :], [], ["pf"])
                    k.cp("vector", pb[:], pf[:], ["pf"], ["pb"])
                    for j in range(2):
                        k.tr(pU[:, j, :], pb[:, j * 128:(j + 1) * 128], identb[:], ["pb", "identb"], ["pU"])
                    k.cp("scalar", pTt[:], pU[:, 0:2, :], ["pU"], ["pTt"])
                    for half in range(2):
                        for j in range(2):
                            k.mm(pGt[:, half, :], pTt[:, j, :], Wple[:, 8 + j, half * 512:(half + 1) * 512],
                                 ["pTt", "Wple"], ["pGt0", "pGt1"], start=(j == 0), stop=(j == 1))
                    k.tt("vector", gt, pGt[:, 0:2, :].rearrange("p a b -> p (a b)"), gt, ALU.mult, ["pGt0", "pGt1", "Uf0"], ["Uf0"])
                    k.tt("vector", x3, gt, acc[:, blk, :], ALU.add, ["Uf0", "acc"], ["Uf0"])
                    o_ = Vf[0][:, blk % 2, :]
                    rmsnorm_bf(nss, nrs, Vf[1][:, 0, :], "Vf1", x3, "Uf0", gfin, "Uf1", o_, "Vf0")
                    k.dma(out[r0:r0 + 128, :], o_, ["Vf0"], ["out"])
                S.emit()
            S.final_wait("sync", ["out"])
            S.emit()
    return nc


def host_consts(T_ALL):
    NBLK = T_ALL // 128
    own0 = T_ALL - T_OWN
    bf = ml_dtypes.bfloat16
    c = {}
    c["c_identb"] = np.eye(128, dtype=np.float32).astype(bf)
    c["c_identf"] = np.eye(128, dtype=np.float32)
    s = np.arange(128)
    c["c_tri"] = (s[:, None] <= s[None, :]).astype(np.float32)
    r = np.arange(512)
    qaug = np.zeros((3, 4, T_OWN), np.float32)
    for h in range(4):
        for g in range(4):
            qaug[0, h, g * 512:(g + 1) * 512] = -SLOPES[h] * 2.0 * (r // 2)
            qaug[1, h, g * 512:(g + 1) * 512] = -SLOPES[h] * (r % 2)
    qaug[2] = NEG
    c["c_qaug"] = qaug.astype(bf)
    bias = np.zeros((128, 4, 4, NBLK), np.float32)
    p = np.arange(128)
    for h in range(4):
        for g in range(4):
            qstart = own0 + 512 * g
            for kb in range(NBLK):
                bias[:, h, g, kb] = SLOPES[h] * (128 * kb + p - qstart)
    c["c_bias"] = bias
    dm = np.zeros((128, 4, 512), np.float32)
    for j in range(4):
        kp = 128 * j + p
        dm[:, j, :] = np.where(kp[:, None] <= r[None, :], 0.0, NEG)
    c["c_dmask"] = dm
    return c


def core_inputs(inputs, core, T_ALL, n_prev_real):
    x = np.asarray(inputs["x"], np.float32)[0]
    own0 = n_prev_real
    xseq = np.zeros((T_ALL, D), np.float32)
    xseq[T_ALL - T_OWN - n_prev_real:] = x[:own0 + T_OWN]
    npad = T_ALL - T_OWN - n_prev_real
    padrow = np.zeros((1, T_ALL), np.float32)
    padrow[0, :npad] = 1.0
    valid = np.ones((T_ALL,), np.float32)
    valid[:npad] = 0.0
    sq = lambda a: np.ascontiguousarray(np.asarray(a, np.float32)[0])
    m = {
        "xseq": xseq,
        "p_own": np.ascontiguousarray(np.asarray(inputs["p"], np.float32)[0, 0, own0:own0 + T_OWN]),
        "padrow": padrow.astype(ml_dtypes.bfloat16),
        "valid": np.ascontiguousarray(valid.reshape(T_ALL // 128, 128).T),
        "w_in": sq(inputs["w_in"]), "norm_mix_g": sq(inputs["norm_mix_g"]),
        "conv_w": np.ascontiguousarray(sq(inputs["conv_w"]).T.reshape(8, 128, 4).transpose(1, 0, 2)),
        "conv_b": np.ascontiguousarray(sq(inputs["conv_b"]).reshape(8, 128).T),
        "b_gates": np.concatenate([sq(inputs["b_igate"]), sq(inputs["b_fgate"])]),
        "lam4": np.stack([sq(inputs["lambda_q1"]), sq(inputs["lambda_k1"]), sq(inputs["lambda_q2"]), sq(inputs["lambda_k2"])]),
        "da_subln_g": sq(inputs["da_subln_g"]), "ml_norm_g": sq(inputs["ml_norm_g"]).reshape(512),
        "w_out": sq(inputs["w_out"]), "norm_ffn_g": sq(inputs["norm_ffn_g"]),
        "w_query": sq(inputs["w_query"]), "sub_keys": sq(inputs["sub_keys"]).reshape(16, 128, 128),
        "expert_u": sq(inputs["expert_u"]), "expert_v": sq(inputs["expert_v"]),
        "norm_ple_g": sq(inputs["norm_ple_g"]), "w_ple_gate": sq(inputs["w_ple_gate"]),
        "w_ple_proj": sq(inputs["w_ple_proj"]), "final_norm_g": np.asarray(inputs["final_norm_g"], np.float32),
    }
    return m


_NC_CACHE = {}


def kernel(**inputs):
    T_ALL = 16384
    n = 8
    if T_ALL not in _NC_CACHE:
        _NC_CACHE[T_ALL] = build(T_ALL)
    nc = _NC_CACHE[T_ALL]
    consts = host_consts(T_ALL)
    in_maps = []
    for c in range(n):
        m = core_inputs(inputs, c, T_ALL, c * T_OWN)
        m.update(consts)
        in_maps.append(m)
    res = run_bass_kernel_spmd(nc, in_maps, core_ids=list(range(n)))
    out = np.concatenate([np.asarray(r["out"], np.float32) for r in res.results], axis=0)
    return out.reshape(1, n * T_OWN, D)
```
